# Optimizing a Trainium2 kernel written in Bass

```python
import jax, jax.numpy as jnp
from jax import lax
import numpy as np

D_MODEL = 2048
BATCH = 8
SEQ = 2048
DEPTH = 2

POOL_WINDOWS = (2, 4, 8, 16)
N_POOL_GROUPS = len(POOL_WINDOWS)
POOL_GROUP_WIDTH = D_MODEL // 8
POOL_WIDTH = N_POOL_GROUPS * POOL_GROUP_WIDTH
HEAD_DIM = 128
N_HEADS = D_MODEL // (2 * HEAD_DIM)
ATTN_WIDTH = N_HEADS * HEAD_DIM
Q_BLOCK = 128
N_BRANCHES = 2
BRANCH_WIDTH = POOL_WIDTH
IN_WIDTH = POOL_WIDTH + 3 * ATTN_WIDTH + N_HEADS
N_EXPERTS = 16
N_EXPERT_GROUPS = 4
EXPERTS_PER_GROUP = N_EXPERTS // N_EXPERT_GROUPS
TOP_K = 2
D_EXPERT = D_MODEL // 2
DISPATCH_BLOCK = 128
N_MOD = 6
EPS = 1e-6

kernel_name = "hybrid_pool_fox_grouped_moe_adaln"


def rmsnorm(x, g):
    x32 = x.astype(jnp.float32)
    y = x32 * lax.rsqrt(jnp.mean(x32 * x32, axis=-1, keepdims=True) + EPS)
    return (y * g.astype(jnp.float32)).astype(x.dtype)


def causal_multiscale_pool(u, w_pool, pool_scale):
    B, S, _ = u.shape
    ug_all = u.reshape(B, S, N_POOL_GROUPS, POOL_GROUP_WIDTH)
    t_idx = jnp.arange(S)
    outs = []
    for g, w in enumerate(POOL_WINDOWS):
        ug = ug_all[:, :, g].astype(jnp.float32)
        cs = jnp.cumsum(ug, axis=1)
        lagged = jnp.pad(cs, ((0, 0), (w, 0), (0, 0)))[:, :S]
        cnt = jnp.minimum(t_idx + 1, w).astype(jnp.float32)[None, :, None]
        outs.append((cs - lagged) / cnt - ug)
    pooled = jnp.stack(outs, axis=2).astype(u.dtype)
    mixed = jnp.einsum('bsgc,gcd->bsgd', pooled, w_pool)
    return mixed.reshape(B, S, POOL_WIDTH) * pool_scale


def forgetting_attention(q, k, v, f_logit):
    B, S, _ = q.shape
    def heads(a):
        return a.reshape(B, S, N_HEADS, HEAD_DIM).transpose(0, 2, 1, 3)
    q, k, v = heads(q), heads(k), heads(v)
    log_f = jax.nn.log_sigmoid(f_logit.astype(jnp.float32)).transpose(0, 2, 1)
    F = jnp.cumsum(log_f, axis=-1)
    nb = S // Q_BLOCK
    q_blocks = q.reshape(B, N_HEADS, nb, Q_BLOCK, HEAD_DIM).transpose(2, 0, 1, 3, 4)
    F_blocks = F.reshape(B, N_HEADS, nb, Q_BLOCK).transpose(2, 0, 1, 3)
    k_pos = jnp.arange(S)
    scale = HEAD_DIM ** -0.5

    def block(args):
        qb, Fq, i = args
        s = jnp.einsum('bhqd,bhkd->bhqk', qb, k, preferred_element_type=jnp.float32) * scale
        s = s + (Fq[..., None] - F[:, :, None, :])
        q_pos = i * Q_BLOCK + jnp.arange(Q_BLOCK)
        s = jnp.where(k_pos[None, :] <= q_pos[:, None], s, -jnp.inf)
        p = jax.nn.softmax(s, axis=-1)
        return jnp.einsum('bhqk,bhkd->bhqd', p.astype(v.dtype), v)

    o = lax.map(block, (q_blocks, F_blocks, jnp.arange(nb)))
    return o.transpose(1, 0, 3, 2, 4).reshape(B, S, ATTN_WIDTH)


def hybrid_mixer(h, w_in, b_forget, w_pool, pool_scale, w_branch, w_gate, b_gate, w_out):
    B, S, D = h.shape
    proj = jnp.einsum('bsd,dn->bsn', h, w_in)
    p0 = POOL_WIDTH
    u_pool = proj[..., :p0]
    q = proj[..., p0:p0 + ATTN_WIDTH]
    k = proj[..., p0 + ATTN_WIDTH:p0 + 2 * ATTN_WIDTH]
    v = proj[..., p0 + 2 * ATTN_WIDTH:p0 + 3 * ATTN_WIDTH]
    f_logit = proj[..., p0 + 3 * ATTN_WIDTH:] + b_forget
    pool_out = causal_multiscale_pool(u_pool, w_pool, pool_scale)
    attn_out = forgetting_attention(q, k, v, f_logit)
    branches = jnp.stack([pool_out, attn_out], axis=2)
    y = jnp.einsum('bsnw,nwd->bsnd', branches, w_branch)
    g = jax.nn.sigmoid(jnp.einsum('bsd,dm->bsm', h, w_gate) + b_gate).reshape(B, S, N_BRANCHES, D)
    merged = jnp.sum(g * y, axis=2)
    return jnp.einsum('bsd,de->bse', merged, w_out)


def grouped_moe(h, w_router, b_router, w_g, w_u, w_d):
    B, S, D = h.shape
    T = B * S
    hf = h.reshape(T, D)
    logits = jnp.einsum('td,de->te', hf, w_router, preferred_element_type=jnp.float32)
    probs = jax.nn.softmax(logits, axis=-1)
    sel = (probs + b_router.astype(jnp.float32)).reshape(T, N_EXPERT_GROUPS, EXPERTS_PER_GROUP)
    group_score = lax.top_k(sel, TOP_K)[0].sum(-1)
    g_star = jnp.argmax(group_score, axis=-1)
    sel_in = sel[jnp.arange(T), g_star]
    _, local = lax.top_k(sel_in, TOP_K)
    expert_idx = g_star[:, None] * EXPERTS_PER_GROUP + local
    gate = jnp.take_along_axis(probs, expert_idx, axis=1)
    gate = gate / jnp.sum(gate, axis=-1, keepdims=True)

    A = T * TOP_K
    e_flat = expert_idx.reshape(A)
    tok_flat = jnp.repeat(jnp.arange(T), TOP_K)
    w_flat = gate.reshape(A)
    order = jnp.argsort(e_flat)
    e_s, tok_s, w_s = e_flat[order], tok_flat[order], w_flat[order]
    counts = jnp.bincount(e_flat, length=N_EXPERTS)
    starts = jnp.cumsum(counts) - counts
    pcounts = (counts + DISPATCH_BLOCK - 1) // DISPATCH_BLOCK * DISPATCH_BLOCK
    pends = jnp.cumsum(pcounts)
    pstarts = pends - pcounts
    dest = pstarts[e_s] + (jnp.arange(A) - starts[e_s])
    n_blocks = (A + DISPATCH_BLOCK - 1) // DISPATCH_BLOCK + N_EXPERTS
    buf = jnp.zeros((n_blocks * DISPATCH_BLOCK, D), h.dtype).at[dest].set(hf[tok_s])
    block_expert = jnp.clip(
        jnp.searchsorted(pends, jnp.arange(n_blocks) * DISPATCH_BLOCK, side='right'), 0, N_EXPERTS - 1)

    def expert_block(args):
        xb, e = args
        a = xb @ w_g[e]
        u = xb @ w_u[e]
        return (jax.nn.silu(a) * u) @ w_d[e]

    y_buf = lax.map(expert_block, (buf.reshape(n_blocks, DISPATCH_BLOCK, D), block_expert))
    y = y_buf.reshape(-1, D)[dest] * w_s[:, None].astype(h.dtype)
    out = jax.ops.segment_sum(y, tok_s, num_segments=T)
    return out.reshape(B, S, D)


def setup_inputs(seed: int = 0) -> dict:
    key = jax.random.key(seed)
    ks = jax.random.split(key, 20)
    nrm = jax.random.normal
    D = D_MODEL
    return {
        "x": nrm(ks[0], (BATCH, SEQ, D), jnp.float32),
        "c": nrm(ks[1], (BATCH, D), jnp.float32),
        "w_ada": nrm(ks[2], (DEPTH, D, N_MOD * D), jnp.float32) * (0.5 * D ** -0.5),
        "b_ada": 0.01 * nrm(ks[3], (DEPTH, N_MOD * D), jnp.float32),
        "norm_mix": 1.0 + 0.1 * nrm(ks[4], (DEPTH, D), jnp.float32),
        "norm_moe": 1.0 + 0.1 * nrm(ks[5], (DEPTH, D), jnp.float32),
        "w_in": nrm(ks[6], (DEPTH, D, IN_WIDTH), jnp.float32) * D ** -0.5,
        "b_forget": 3.0 + 0.5 * nrm(ks[7], (DEPTH, N_HEADS), jnp.float32),
        "w_pool": nrm(ks[8], (DEPTH, N_POOL_GROUPS, POOL_GROUP_WIDTH, POOL_GROUP_WIDTH), jnp.float32) * POOL_GROUP_WIDTH ** -0.5,
        "pool_scale": 1.0 + 0.1 * nrm(ks[9], (DEPTH, POOL_WIDTH), jnp.float32),
        "w_branch": nrm(ks[10], (DEPTH, N_BRANCHES, BRANCH_WIDTH, D), jnp.float32) * BRANCH_WIDTH ** -0.5,
        "w_gate": nrm(ks[11], (DEPTH, D, N_BRANCHES * D), jnp.float32) * D ** -0.5,
        "b_gate": 0.01 * nrm(ks[12], (DEPTH, N_BRANCHES * D), jnp.float32),
        "w_out": nrm(ks[13], (DEPTH, D, D), jnp.float32) * D ** -0.5,
        "w_router": nrm(ks[14], (D, N_EXPERTS), jnp.float32) * D ** -0.5,
        "b_router": 0.01 * nrm(ks[15], (N_EXPERTS,), jnp.float32),
        "w_exp_gate": nrm(ks[16], (DEPTH, N_EXPERTS, D, D_EXPERT), jnp.float32) * D ** -0.5,
        "w_exp_up": nrm(ks[17], (DEPTH, N_EXPERTS, D, D_EXPERT), jnp.float32) * D ** -0.5,
        "w_exp_down": nrm(ks[18], (DEPTH, N_EXPERTS, D_EXPERT, D), jnp.float32) * D_EXPERT ** -0.5,
        "norm_final": 1.0 + 0.1 * nrm(ks[19], (D,), jnp.float32),
    }


def reference(x, c, w_ada, b_ada, norm_mix, norm_moe, w_in, b_forget, w_pool, pool_scale,
              w_branch, w_gate, b_gate, w_out, w_router, b_router, w_exp_gate, w_exp_up,
              w_exp_down, norm_final):
    c_act = jax.nn.silu(c)
    for l in range(DEPTH):
        mod = (jnp.einsum('bd,dm->bm', c_act, w_ada[l]) + b_ada[l])[:, None, :]
        shift_a, scale_a, gate_a, shift_m, scale_m, gate_m = jnp.split(mod, N_MOD, axis=-1)
        h = rmsnorm(x, norm_mix[l]) * (1.0 + scale_a) + shift_a
        x = x + gate_a * hybrid_mixer(h, w_in[l], b_forget[l], w_pool[l], pool_scale[l],
                                      w_branch[l], w_gate[l], b_gate[l], w_out[l])
        h = rmsnorm(x, norm_moe[l]) * (1.0 + scale_m) + shift_m
        x = x + gate_m * grouped_moe(h, w_router, b_router, w_exp_gate[l], w_exp_up[l], w_exp_down[l])
    return rmsnorm(x, norm_final)
```

```python
import contextlib
import numpy as np
import concourse.bass as bass
import concourse.mybir as mybir
from concourse.bass_utils import run_bass_kernel_spmd

F32 = mybir.dt.float32
BF16 = mybir.dt.bfloat16
AF = mybir.ActivationFunctionType
ALU = mybir.AluOpType
AX = mybir.AxisListType

S = 2048
D = 2048
NT = 16
KC = 16
INW = 4104
NE = 16
DE = 1024
EPS = 1e-6
NDS = 40
NBLK = 48
I32 = mybir.dt.int32
BIGIDX = 1000000.0
SPARSE = True
SKIP = True


class Slot:
    __slots__ = ("w", "r")

    def __init__(self):
        self.w = None
        self.r = {}


class Prog:
    def __init__(self, depth=2, dbg=False, stop_after=None, nexp=NE):
        self.depth = depth
        self.dbg = dbg
        self.stop_after = stop_after
        self.nexp = nexp
        nc = bass.Bass("TRN2", target_bir_lowering=False)
        self.nc = nc
        self.es = contextlib.ExitStack()
        self.eng = dict(pe=nc.tensor, act=nc.scalar, dve=nc.vector, pool=nc.gpsimd, sp=nc.sync)
        self.semh = {}
        for e in self.eng:
            self.semh[e] = self.es.enter_context(nc.semaphore("p_" + e))
        for i in range(NDS):
            self.semh[f"d{i}"] = self.es.enter_context(nc.semaphore(f"dq{i}"))
        self.cnt = {e: 0 for e in self.eng}
        self.dcnt = [0] * NDS
        self.dnext = 0
        self._dq = {}
        self.seen = {e: {} for e in self.eng}
        self.pend = {e: ([], []) for e in self.eng}
        self.last = {}

    def _wait(self, e, deps):
        best = {}
        for d in deps:
            if d is not None:
                if best.get(d[0], 0) < d[1]:
                    best[d[0]] = d[1]
        for s, v in best.items():
            if self.seen[e].get(s, 0) < v:
                self.eng[e].wait_ge(self.semh[s], v)
                self.seen[e][s] = v

    def _deps(self, reads, writes, extra):
        deps = list(extra)
        for s in reads:
            deps.append(s.w)
        for s in writes:
            deps.append(s.w)
            deps.extend(s.r.items())
        return deps

    def op(self, e, build, reads=(), writes=(), extra=(), signal=True):
        self._wait(e, self._deps(reads, writes, extra))
        ins = build(self.eng[e])
        pr, pw = self.pend[e]
        pr.extend(reads)
        pw.extend(writes)
        if not signal:
            return None
        self.cnt[e] += 1
        ins.then_inc(self.semh[e], 1)
        tok = (e, self.cnt[e])
        for s in pr:
            if s.r.get(e, 0) < tok[1]:
                s.r[e] = tok[1]
        for s in pw:
            s.w = tok
            s.r = {}
        pr.clear()
        pw.clear()
        self.last[e] = tok
        return tok

    def dma(self, q, out, in_, reads=(), writes=(), extra=(), **kw):
        self._wait(q, self._deps(reads, writes, extra))
        i = self._next_dsem(q)
        self.dcnt[i] += 1
        self.eng[q].dma_start(out=out, in_=in_, **kw).then_inc(self.semh[f"d{i}"], 16)
        tok = (f"d{i}", 16 * self.dcnt[i])
        for s in reads:
            if s.r.get(tok[0], 0) < tok[1]:
                s.r[tok[0]] = tok[1]
        for s in writes:
            s.w = tok
            s.r = {}
        return tok

    def _next_dsem(self, q):
        lo, n = (0, 16) if q == "sp" else (16, NDS - 16)
        st = self._dq.setdefault(q, 0)
        self._dq[q] = (st + 1) % n
        return lo + st

    def barrier(self):
        toks = [t for t in self.last.values()]
        toks += [(f"d{i}", 16 * self.dcnt[i]) for i in range(NDS) if self.dcnt[i] > 0]
        for e in self.eng:
            self._wait(e, toks)

    def sb(self, ph, name, shape, dt):
        self._n = getattr(self, "_n", 0) + 1
        return ph.enter_context(self.nc.sbuf_tensor(f"{name}_{self._n}", shape, dt))

    def dram(self, name, shape, dt, kind):
        return self.nc.dram_tensor(name, shape, dt, kind=kind).ap()

    def build(self):
        nc = self.nc
        I = "ExternalInput"
        self.x = self.dram("x", [S, D], F32, I)
        self.c = self.dram("c", [1, D], F32, I)
        self.w_ada = self.dram("w_ada", [2, D, 6 * D], F32, I)
        self.b_ada = self.dram("b_ada", [2, 6 * D], F32, I)
        self.norm_mix = self.dram("norm_mix", [2, D], F32, I)
        self.norm_moe = self.dram("norm_moe", [2, D], F32, I)
        self.w_in = self.dram("w_in", [2, D, INW], F32, I)
        self.b_forget = self.dram("b_forget", [2, 8], F32, I)
        self.w_pool = self.dram("w_pool", [2, 4, 256, 256], F32, I)
        self.pool_scale = self.dram("pool_scale", [2, 1024], F32, I)
        self.w_branch = self.dram("w_branch", [2, 2, 1024, D], F32, I)
        self.w_gate = self.dram("w_gate", [2, D, 2 * D], F32, I)
        self.b_gate = self.dram("b_gate", [2, 2 * D], F32, I)
        self.w_out = self.dram("w_out", [2, D, D], F32, I)
        self.w_router = self.dram("w_router", [D, NE], F32, I)
        self.b_router = self.dram("b_router", [1, NE], F32, I)
        small = self.stop_after is not None and (self.stop_after == "mod" or self.stop_after.startswith("mixer") or self.stop_after.startswith("gate"))
        self.small_exp = small
        eshape = [2, NE, 1, 8] if small else None
        self.w_eg = self.dram("w_exp_gate", eshape or [2, NE, D, DE], F32, I)
        self.w_eu = self.dram("w_exp_up", eshape or [2, NE, D, DE], F32, I)
        self.w_ed = self.dram("w_exp_down", eshape or [2, NE, DE, D], F32, I)
        self.norm_final = self.dram("norm_final", [1, D], F32, I)
        self.out = self.dram("out", [S, D], F32, "ExternalOutput")
        sk = "ExternalOutput" if self.dbg else "Internal"
        self.xres = self.dram("xres", [S, D], F32, sk)
        self.mod_d = self.dram("mod_d", [2, 6 * D], F32, sk)
        self.h2T_d = self.dram("h2T_d", [128, KC, S], BF16, sk)
        self.mT_d = self.dram("mT_d", [NT, 128, KC, 128], BF16, sk)
        self.gwT_d = self.dram("gwT_d", [NE, S], F32, sk)
        self.buf_d = self.dram("buf_d", [NBLK * 128, D], BF16, "Internal")
        self.ybuf_d = self.dram("ybuf_d", [NBLK * 128, D], F32, "Internal")
        if self.dbg:
            self.dbg_idx = self.dram("dbg_idx", [128, 4 * NT + 2 * NBLK], F32, sk)
        if self.dbg:
            self.dbg_h = self.dram("dbg_h", [128, KC, S], BF16, sk)
            self.dbg_pool = self.dram("dbg_pool", [128, 8, S], BF16, sk)
            self.dbg_attn = self.dram("dbg_attn", [128, 8, S], BF16, sk)
            self.dbg_F = self.dram("dbg_F", [128, NT, 8], F32, sk)

        with self.es:
            es = self.es
            self.pb = [es.enter_context(nc.psum_tensor(f"pb{i}", [128, 512], F32)) for i in range(8)]
            self.pbs = [Slot() for _ in range(8)]
            self.ident32 = self.sb(es, "ident32", [128, 128], F32)
            self.ones32 = self.sb(es, "ones32", [128, 128], F32)
            self.uinc32 = self.sb(es, "uinc32", [128, 128], F32)
            self.onesbf = self.sb(es, "onesbf", [128, 128], BF16)
            self.tribf = self.sb(es, "tribf", [128, 128], BF16)
            self.s_const = Slot()
            g = nc.gpsimd
            self.op("pool", lambda E: E.memset(self.ident32[:], 1.0), writes=(self.s_const,))
            self.op("pool", lambda E: E.affine_select(out=self.ident32[:], in_=self.ident32[:], pattern=[[-1, 128]],
                                                      compare_op=ALU.is_equal, fill=0.0, base=0, channel_multiplier=1),
                    writes=(self.s_const,))
            self.op("pool", lambda E: E.memset(self.ones32[:], 1.0), writes=(self.s_const,))
            self.op("pool", lambda E: E.memset(self.onesbf[:], 1.0), writes=(self.s_const,))
            self.op("pool", lambda E: E.memset(self.uinc32[:], 1.0), writes=(self.s_const,))
            self.op("pool", lambda E: E.affine_select(out=self.uinc32[:], in_=self.uinc32[:], pattern=[[1, 128]],
                                                      compare_op=ALU.is_ge, fill=0.0, base=0, channel_multiplier=-1),
                    writes=(self.s_const,))
            self.op("pool", lambda E: E.tensor_copy(self.tribf[:], self.uinc32[:]), writes=(self.s_const,))
            self.identbf = self.sb(es, "identbf", [128, 128], BF16)
            self.op("pool", lambda E: E.tensor_copy(self.identbf[:], self.ident32[:]), writes=(self.s_const,))
            self.ustr32 = self.sb(es, "ustr32", [128, 128], F32)
            self.ustrbf = self.sb(es, "ustrbf", [128, 128], BF16)
            self.op("pool", lambda E: E.memset(self.ustr32[:], 1.0), writes=(self.s_const,))
            self.op("pool", lambda E: E.affine_select(out=self.ustr32[:], in_=self.ustr32[:], pattern=[[1, 128]],
                                                      compare_op=ALU.is_gt, fill=0.0, base=0, channel_multiplier=-1),
                    writes=(self.s_const,))
            self.op("pool", lambda E: E.tensor_copy(self.ustrbf[:], self.ustr32[:]), writes=(self.s_const,))
            self.barrier()

            self.phase_mod()
            if self.stop_after == "mod":
                return self._finish()
            for l in range(self.depth):
                x_src = self.x if l == 0 else self.xres
                self.layer_mixer(l, x_src)
                if self.stop_after == f"mixer{l}":
                    return self._finish()
                self.layer_moe(l)
                if self.stop_after in (f"moe{l}", f"gate{l}"):
                    return self._finish()
            if not SPARSE:
                self.phase_final()
            return self._finish()

    def _finish(self):
        self.barrier()
        return self.nc

    def phase_mod(self):
        nc = self.nc
        with contextlib.ExitStack() as ph:
            cT = self.sb(ph, "cT", [128, 16], F32)
            cact = self.sb(ph, "cact", [128, 16], BF16)
            s_c, s_ca, s_b, s_m = Slot(), Slot(), Slot(), Slot()
            self.dma("sp", cT[:], self.c.rearrange("o (k p) -> p (o k)", p=128), writes=(s_c,),
                     allow_slow_non_contiguous=True)
            self.op("act", lambda E: E.activation(out=cact[:], in_=cT[:], func=AF.Silu), reads=(s_c,), writes=(s_ca,))
            wring = [self.sb(ph, f"wada{i}", [128, 3072], BF16) for i in range(4)]
            wsl = [Slot() for _ in range(4)]
            brow = self.sb(ph, "brow", [1, 6 * D], F32)
            mrow = self.sb(ph, "mrow", [1, 6 * D], F32)
            i = 0
            for l in range(self.depth):
                self.dma("sp", brow[:], self.b_ada[l:l + 1, :], writes=(s_b,))
                for grp in range(4):
                    for k in range(16):
                        r = i % 4
                        i += 1
                        self.dma("pool", wring[r][:], self.w_ada[l, k * 128:(k + 1) * 128, grp * 3072:(grp + 1) * 3072],
                                 writes=(wsl[r],))
                        for n in range(6):
                            self.op("pe", lambda E: E.matmul(self.pb[n][0:1, :], cact[:, k:k + 1],
                                                             wring[r][:, n * 512:(n + 1) * 512],
                                                             start=(k == 0), stop=(k == 15)),
                                    reads=(s_ca, wsl[r]), writes=(self.pbs[n],), signal=(n == 5))
                    for n in range(6):
                        col = grp * 3072 + n * 512
                        self.op("dve", lambda E: E.tensor_tensor(out=mrow[0:1, col:col + 512], in0=self.pb[n][0:1, :],
                                                                 in1=brow[0:1, col:col + 512], op=ALU.add),
                                reads=(self.pbs[n], s_b), writes=(s_m,))
                self.dma("sp", self.mod_d[l:l + 1, :], mrow[:], reads=(s_m,))
        self.barrier()

    def phase_norm(self, ph, l, x_src, gnorm_row, off_shift, off_scale, hT, s_hT, router=None, tm_out=None):
        A = self.sb(ph, "A", [128, D], F32)
        B = self.sb(ph, "B", [128, D], F32)
        s_A, s_B = Slot(), Slot()
        self.dma("sp", A[:], self.mod_d[l:l + 1, off_scale:off_scale + D].partition_broadcast(128), writes=(s_A,))
        self.dma("sp", B[:], gnorm_row.partition_broadcast(128), writes=(s_B,))
        self.op("dve", lambda E: E.scalar_tensor_tensor(out=A[:], in0=A[:], scalar=1.0, in1=B[:], op0=ALU.add,
                                                        op1=ALU.mult), reads=(s_B,), writes=(s_A,))
        self.dma("sp", B[:], self.mod_d[l:l + 1, off_shift:off_shift + D].partition_broadcast(128), writes=(s_B,))
        xt = [self.sb(ph, f"xt{i}", [128, D], F32) for i in range(2)]
        s_xt = [Slot(), Slot()]
        junk = self.sb(ph, "junk", [128, D], BF16)
        s_junk = Slot()
        ss = [self.sb(ph, f"ss{i}", [128, 1], F32) for i in range(2)]
        s_ss = [Slot(), Slot()]
        t1 = [self.sb(ph, f"t1{i}", [128, D], F32) for i in range(2)]
        s_t1 = [Slot(), Slot()]
        if router is not None:
            hT32 = self.sb(ph, "hlo", [128, KC, 128], BF16)
            s_h32 = Slot()
            wr, s_wr, lg, s_lg = router
        loads = {}

        def load(t):
            loads[t] = self.dma("sp", xt[t % 2][:], x_src[t * 128:(t + 1) * 128, :], writes=(s_xt[t % 2],))

        load(0)
        bank = 0
        for t in range(NT):
            b = t % 2
            if t + 1 < NT:
                load(t + 1)
            self.op("act", lambda E: E.activation(out=junk[:], in_=xt[b][:], func=AF.Square, accum_out=ss[b][:]),
                    reads=(s_xt[b],), writes=(s_junk, s_ss[b]))
            self.op("dve", lambda E: E.tensor_scalar(out=ss[b][:], in0=ss[b][:], scalar1=1.0 / D, scalar2=EPS,
                                                     op0=ALU.mult, op1=ALU.add), writes=(s_ss[b],))
            self.op("act", lambda E: E.activation(out=ss[b][:], in_=ss[b][:], func=AF.Sqrt), writes=(s_ss[b],))
            self.op("dve", lambda E: E.reciprocal(out=ss[b][:], in_=ss[b][:]), writes=(s_ss[b],))
            self.op("dve", lambda E: E.scalar_tensor_tensor(out=t1[b][:], in0=xt[b][:], scalar=ss[b][:, 0:1], in1=A[:],
                                                            op0=ALU.mult, op1=ALU.mult),
                    reads=(s_xt[b], s_ss[b], s_A), writes=(s_t1[b],))
            self.op("dve", lambda E: E.tensor_tensor(out=t1[b][:], in0=t1[b][:], in1=B[:], op=ALU.add),
                    reads=(s_B,), writes=(s_t1[b],))
            hc = slice(0, 128) if tm_out is not None else slice(t * 128, (t + 1) * 128)
            if tm_out is not None:
                h2tm, s_h2tm = tm_out
                self.op("pool", lambda E: E.tensor_copy(h2tm[:, t, :], t1[b][:]), reads=(s_t1[b],), writes=(s_h2tm,))
            for g in range(4):
                pbk = 2 * (t % 2) + (g % 2) if router is not None else (4 * (t % 2) + g)
                for j in range(4):
                    k = g * 4 + j
                    self.op("pe", lambda E: E.transpose(self.pb[pbk][:, j * 128:(j + 1) * 128],
                                                        t1[b][:, k * 128:(k + 1) * 128], self.ident32[:]),
                            reads=(s_t1[b], self.s_const), writes=(self.pbs[pbk],), signal=(j == 3))
                src = self.pb[pbk][:].rearrange("p (j t) -> p j t", j=4)
                self.op("act", lambda E: E.activation(out=hT[:, g * 4:(g + 1) * 4, hc], in_=src,
                                                      func=AF.Copy), reads=(self.pbs[pbk],), writes=(s_hT,))
                if router is not None:
                    self.op("dve", lambda E: E.tensor_tensor(out=hT32[:, g * 4:(g + 1) * 4, :], in0=src,
                                                             in1=hT[:, g * 4:(g + 1) * 4, hc],
                                                             op=ALU.subtract),
                            reads=(self.pbs[pbk], s_hT), writes=(s_h32,))
            if router is not None:
                rb = 4 + (t % 4)
                for k in range(KC):
                    hi = hT[:, k, hc]
                    lo = hT32[:, k, :]
                    for ii, (lh, rw) in enumerate(((hi, 0), (lo, 0), (hi, 1))):
                        self.op("pe", lambda E: E.matmul(self.pb[rb][:, 0:NE], lh, wr[:, rw, k, :],
                                                         start=(k == 0 and ii == 0), stop=(k == KC - 1 and ii == 2)),
                                reads=(s_h32, s_wr, s_hT), writes=(self.pbs[rb],), signal=(k == KC - 1 and ii == 2))
                self.op("dve", lambda E: E.tensor_copy(lg[:, t, :], self.pb[rb][:, 0:NE]),
                        reads=(self.pbs[rb],), writes=(s_lg,))

    def layer_mixer(self, l, x_src):
        nc = self.nc
        with contextlib.ExitStack() as ph:
            hT = self.sb(ph, "hT", [128, KC, S], BF16)
            s_hT = Slot()
            with contextlib.ExitStack() as ph2:
                self.phase_norm(ph2, l, x_src, self.norm_mix[l:l + 1, :], 0, D, hT, s_hT)
            self.barrier()
            if self.dbg and l == 0:
                self.dma("sp", self.dbg_h, hT[:], reads=(s_hT,))
            pool_out = self.sb(ph, "pool_out", [128, 8, S], BF16)
            attn_out = self.sb(ph, "attn_out", [128, 8, S], BF16)
            s_po, s_ao = Slot(), Slot()
            with contextlib.ExitStack() as ph2:
                self.phase_pool(ph2, l, hT, s_hT, pool_out, s_po)
            self.barrier()
            with contextlib.ExitStack() as ph2:
                self.phase_attn(ph2, l, hT, s_hT, attn_out, s_ao)
            self.barrier()
            if self.dbg and l == 0:
                self.dma("sp", self.dbg_pool, pool_out[:], reads=(s_po,))
                self.dma("sp", self.dbg_attn, attn_out[:], reads=(s_ao,))
            with contextlib.ExitStack() as ph2:
                self.phase_merge(ph2, l, hT, s_hT, pool_out, s_po, attn_out, s_ao)
            self.barrier()
        with contextlib.ExitStack() as ph:
            self.phase_wout(ph, l, x_src)
        self.barrier()

    def wblock(self, dst, src_cols):
        return src_cols.rearrange("(k p) n -> p k n", p=128)

    def phase_pool(self, ph, l, hT, s_hT, pool_out, s_po):
        PADW = 16
        pooled = self.sb(ph, "pooled", [128, 8, S], BF16)
        s_pl = Slot()
        wblk = [self.sb(ph, f"wblk{i}", [128, KC, 128], BF16) for i in range(3)]
        s_wb = [Slot() for _ in range(3)]
        u32 = self.sb(ph, "u32", [128, PADW + S], F32)
        sA = self.sb(ph, "sA", [128, PADW + S], F32)
        sB = self.sb(ph, "sB", [128, PADW + S], F32)
        s_u, s_sA, s_sB = Slot(), Slot(), Slot()
        invc = self.sb(ph, "invc", [128, 16], F32)
        tmpf = self.sb(ph, "tmpf", [128, 16], F32)
        s_inv, s_tmpf = Slot(), Slot()
        psc = self.sb(ph, "psc", [128, 8], F32)
        s_psc = Slot()
        wp = self.sb(ph, "wp", [128, 8, 256], BF16)
        s_wp = Slot()
        self.dma("sp", psc[:], self.pool_scale[l:l + 1, :].rearrange("o (c p) -> p (o c)", p=128), writes=(s_psc,),
                 allow_slow_non_contiguous=True)
        self.dma("pool", wp[:], self.w_pool[l].rearrange("g (i p) d -> p (g i) d", p=128), writes=(s_wp,))
        for buf, sl in ((u32, s_u), (sA, s_sA), (sB, s_sB)):
            self.op("dve", lambda E: E.memset(buf[:, 0:PADW], 0.0), writes=(sl,))
        for tcol in range(16):
            self.op("dve", lambda E: E.memset(invc[:, tcol:tcol + 1], 1.0 / (tcol + 1)), writes=(s_inv,))
        for c in range(8):
            r = c % 3
            self.dma("pool", wblk[r][:], self.w_in[l, :, c * 128:(c + 1) * 128].rearrange("(k p) n -> p k n", p=128),
                     writes=(s_wb[r],))
            for tt in range(4):
                bk = (c * 4 + tt) % 8
                for k in range(KC):
                    self.op("pe", lambda E: E.matmul(self.pb[bk][:], wblk[r][:, k, :], hT[:, k, tt * 512:(tt + 1) * 512],
                                                     start=(k == 0), stop=(k == KC - 1)),
                            reads=(s_wb[r], s_hT), writes=(self.pbs[bk],), signal=(k == KC - 1))
                self.op("act", lambda E: E.activation(out=u32[:, PADW + tt * 512:PADW + (tt + 1) * 512], in_=self.pb[bk][:],
                                                      func=AF.Copy), reads=(self.pbs[bk],), writes=(s_u,))
            g = c // 2
            w = 2 << g
            cur, s_cur = u32, s_u
            nxt = [(sA, s_sA), (sB, s_sB)]
            for i in range(g + 1):
                sh = 1 << i
                dst, s_dst = nxt[i % 2]
                self.op("dve", lambda E: E.tensor_tensor(out=dst[:, PADW:], in0=cur[:, PADW:],
                                                         in1=cur[:, PADW - sh:PADW - sh + S], op=ALU.add),
                        reads=(s_cur,), writes=(s_dst,))
                cur, s_cur = dst, s_dst
            self.op("dve", lambda E: E.scalar_tensor_tensor(out=pooled[:, c, :], in0=cur[:, PADW:], scalar=1.0 / w,
                                                            in1=u32[:, PADW:], op0=ALU.mult, op1=ALU.subtract),
                    reads=(s_cur, s_u), writes=(s_pl,))
            self.op("dve", lambda E: E.tensor_tensor(out=tmpf[:, 0:w], in0=cur[:, PADW:PADW + w], in1=invc[:, 0:w],
                                                     op=ALU.mult), reads=(s_cur, s_inv), writes=(s_tmpf,))
            self.op("dve", lambda E: E.tensor_tensor(out=pooled[:, c, 0:w], in0=tmpf[:, 0:w], in1=u32[:, PADW:PADW + w],
                                                     op=ALU.subtract), reads=(s_tmpf, s_u), writes=(s_pl,))
        for g in range(4):
            for oc in range(2):
                for tt in range(4):
                    bk = (g * 8 + oc * 4 + tt) % 8
                    for ic in range(2):
                        self.op("pe", lambda E: E.matmul(self.pb[bk][:], wp[:, g * 2 + ic, oc * 128:(oc + 1) * 128],
                                                         pooled[:, g * 2 + ic, tt * 512:(tt + 1) * 512],
                                                         start=(ic == 0), stop=(ic == 1)),
                                reads=(s_wp, s_pl), writes=(self.pbs[bk],), signal=(ic == 1))
                    cc = g * 2 + oc
                    self.op("act", lambda E: E.activation(out=pool_out[:, cc, tt * 512:(tt + 1) * 512], in_=self.pb[bk][:],
                                                          func=AF.Identity, scale=psc[:, cc:cc + 1]),
                            reads=(self.pbs[bk], s_psc), writes=(s_po,))

    def phase_attn(self, ph, l, hT, s_hT, attn_out, s_ao):
        scale = 128 ** -0.5
        V = self.sb(ph, "V", [128, NT, 1024], BF16)
        s_V = Slot()
        Fp = self.sb(ph, "Fp", [128, NT, 8], F32)
        s_Fp = Slot()
        with contextlib.ExitStack() as p3:
            wv = self.sb(p3, "wv", [128, KC, 1024], BF16)
            wf = self.sb(p3, "wf", [128, KC, 8], BF16)
            s_wv, s_wf = Slot(), Slot()
            for q in range(4):
                self.dma("pool", wv[:, q * 4:(q + 1) * 4, :],
                         self.w_in[l, q * 512:(q + 1) * 512, 3072:4096].rearrange("(k p) n -> p k n", p=128),
                         writes=(s_wv,))
            self.dma("pool", wf[:], self.w_in[l, :, 4096:4104].rearrange("(k p) n -> p k n", p=128), writes=(s_wf,))
            bfb = self.sb(p3, "bfb", [128, 8], F32)
            s_bfb = Slot()
            self.dma("sp", bfb[:], self.b_forget[l:l + 1, :].partition_broadcast(128), writes=(s_bfb,))
            z = self.sb(p3, "z", [128, NT, 8], F32)
            s_z = Slot()
            for t in range(NT):
                for k in range(KC):
                    for hh in range(2):
                        bk = (t * 3 + hh) % 8
                        self.op("pe", lambda E: E.matmul(self.pb[bk][:], hT[:, k, t * 128:(t + 1) * 128],
                                                         wv[:, k, hh * 512:(hh + 1) * 512],
                                                         start=(k == 0), stop=(k == KC - 1)),
                                reads=(s_hT, s_wv), writes=(self.pbs[bk],), signal=(k == KC - 1))
                for hh in range(2):
                    bk = (t * 3 + hh) % 8
                    if hh == 0:
                        self.op("act", lambda E: E.activation(out=V[:, t, hh * 512:(hh + 1) * 512], in_=self.pb[bk][:],
                                                              func=AF.Copy), reads=(self.pbs[bk],), writes=(s_V,))
                    else:
                        self.op("dve", lambda E: E.tensor_copy(V[:, t, hh * 512:(hh + 1) * 512], self.pb[bk][:]),
                                reads=(self.pbs[bk],), writes=(s_V,))
                bk = (t * 3 + 2) % 8
                for k in range(KC):
                    self.op("pe", lambda E: E.matmul(self.pb[bk][:, 0:8], hT[:, k, t * 128:(t + 1) * 128], wf[:, k, :],
                                                     start=(k == 0), stop=(k == KC - 1)),
                            reads=(s_hT, s_wf), writes=(self.pbs[bk],), signal=(k == KC - 1))
                self.op("dve", lambda E: E.tensor_tensor(out=z[:, t, :], in0=self.pb[bk][:, 0:8], in1=bfb[:], op=ALU.add),
                        reads=(self.pbs[bk], s_bfb), writes=(s_z,))
            zf = z[:].rearrange("p t h -> p (t h)")
            self.op("act", lambda E: E.activation(out=zf, in_=zf, func=AF.Exp, scale=-1.0), writes=(s_z,))
            self.op("act", lambda E: E.activation(out=zf, in_=zf, func=AF.Ln, bias=1.0, scale=1.0), writes=(s_z,))
            self.op("pe", lambda E: E.matmul(self.pb[0][:, 0:128], self.uinc32[:], zf, start=True, stop=True),
                    reads=(s_z, self.s_const), writes=(self.pbs[0],))
            self.op("pe", lambda E: E.matmul(self.pb[1][:, 0:128], self.ones32[:], zf, start=True, stop=True),
                    reads=(s_z, self.s_const), writes=(self.pbs[1],))
            tot = self.sb(p3, "tot", [128, NT, 8], F32)
            toff = self.sb(p3, "toff", [128, NT, 8], F32)
            s_tot, s_toff = Slot(), Slot()
            self.op("dve", lambda E: E.tensor_copy(tot[:].rearrange("p t h -> p (t h)"), self.pb[1][:, 0:128]),
                    reads=(self.pbs[1],), writes=(s_tot,))
            self.op("dve", lambda E: E.memset(toff[:, 0, :], 0.0), writes=(s_toff,))
            for t in range(1, NT):
                self.op("dve", lambda E: E.tensor_tensor(out=toff[:, t, :], in0=toff[:, t - 1, :], in1=tot[:, t - 1, :],
                                                         op=ALU.add), reads=(s_tot,), writes=(s_toff,))
            self.op("dve", lambda E: E.tensor_tensor(out=Fp[:].rearrange("p t h -> p (t h)"), in0=self.pb[0][:, 0:128],
                                                     in1=toff[:].rearrange("p t h -> p (t h)"), op=ALU.add),
                    reads=(self.pbs[0], s_toff), writes=(s_Fp,))
            if self.dbg and l == 0:
                self.dma("sp", self.dbg_F, Fp[:], reads=(s_Fp,))
            self.barrier()
        wqk = [self.sb(ph, f"wqk{i}", [128, 2, KC, 128], BF16) for i in range(2)]
        s_wqk = [Slot(), Slot()]
        qT = self.sb(ph, "qT", [128, S], BF16)
        kT = self.sb(ph, "kT", [128, S], BF16)
        s_q, s_k = Slot(), Slot()
        Ftb = self.sb(ph, "Ftb", [128, S], F32)
        s_Ftb = Slot()
        diag = [self.sb(ph, f"diag{i}", [128, 128], F32) for i in range(2)]
        s_diag = [Slot(), Slot()]
        tmp = [self.sb(ph, f"atmp{i}", [128, 512], F32) for i in range(2)]
        s_tmp = [Slot() for _ in range(2)]
        P = [self.sb(ph, f"P{i}", [128, 512], BF16) for i in range(4)]
        s_P = [Slot() for _ in range(4)]
        self._att_it = 0
        rden = self.sb(ph, "rden", [128, 512], F32)
        s_rden = Slot()
        ip = 0
        it = 0
        for h in range(8):
            r = h % 2
            self.dma("pool", wqk[r][:, 0],
                     self.w_in[l, :, 1024 + h * 128:1024 + (h + 1) * 128].rearrange("(k p) n -> p k n", p=128),
                     writes=(s_wqk[r],))
            self.dma("pool", wqk[r][:, 1],
                     self.w_in[l, :, 2048 + h * 128:2048 + (h + 1) * 128].rearrange("(k p) n -> p k n", p=128),
                     writes=(s_wqk[r],))
            for which, dst, s_dst in ((0, qT, s_q), (1, kT, s_k)):
                for tt in range(4):
                    bk = tt % 2
                    for k in range(KC):
                        self.op("pe", lambda E: E.matmul(self.pb[bk][:], wqk[r][:, which, k, :],
                                                         hT[:, k, tt * 512:(tt + 1) * 512],
                                                         start=(k == 0), stop=(k == KC - 1)),
                                reads=(s_wqk[r], s_hT), writes=(self.pbs[bk],), signal=(k == KC - 1))
                    if which == 0:
                        self.op("act", lambda E: E.activation(out=dst[:, tt * 512:(tt + 1) * 512], in_=self.pb[bk][:],
                                                              func=AF.Copy), reads=(self.pbs[bk],), writes=(s_dst,))
                    else:
                        self.op("dve", lambda E: E.tensor_copy(dst[:, tt * 512:(tt + 1) * 512], self.pb[bk][:]),
                                reads=(self.pbs[bk],), writes=(s_dst,))
            for t in range(NT):
                d = t % 2
                bk = (t // 4) % 2
                self.op("dve", lambda E: E.tensor_scalar(out=diag[d][:], in0=self.ident32[:], scalar1=Fp[:, t, h:h + 1],
                                                         scalar2=-1.0, op0=ALU.mult, op1=ALU.mult),
                        reads=(s_Fp, self.s_const), writes=(s_diag[d],))
                self.op("pe", lambda E: E.matmul(self.pb[bk][:, (t % 4) * 128:(t % 4 + 1) * 128], self.ones32[:], diag[d][:],
                                                 start=True, stop=True),
                        reads=(s_diag[d], self.s_const), writes=(self.pbs[bk],), signal=True)
                if t % 4 == 3:
                    self.op("act", lambda E: E.activation(out=Ftb[:, (t - 3) * 128:(t + 1) * 128], in_=self.pb[bk][:],
                                                          func=AF.Copy), reads=(self.pbs[bk],), writes=(s_Ftb,))
            tiles = []
            for j in range(4):
                nk = 4 * j + 4
                for kc in range(nk):
                    tiles.append((j, kc, nk))
            SB = (2, 3, 4, 7)
            LA = 3
            meta = {}

            def emit_S(i):
                j, kc, nk = tiles[i]
                q0 = 0 if kc < 4 * j else 128 * (kc - 4 * j)
                ncol = 512 - q0
                qs = j * 512 + q0
                nonlocal_it = self._att_it
                self._att_it += 1
                bS = SB[nonlocal_it % 4]
                tb = nonlocal_it % 2
                pi = nonlocal_it % 4
                meta[i] = (q0, ncol, pi)
                self.op("pe", lambda E: E.matmul(self.pb[bS][:, 0:ncol], kT[:, kc * 128:(kc + 1) * 128],
                                                 qT[:, qs:qs + ncol], start=True, stop=True),
                        reads=(s_k, s_q), writes=(self.pbs[bS],))
                self.op("dve", lambda E: E.scalar_tensor_tensor(out=tmp[tb][:, 0:ncol], in0=self.pb[bS][:, 0:ncol],
                                                                scalar=scale, in1=Ftb[:, qs:qs + ncol],
                                                                op0=ALU.mult, op1=ALU.add),
                        reads=(self.pbs[bS], s_Ftb), writes=(s_tmp[tb],))
                self.op("act", lambda E: E.activation(out=P[pi][:, 0:ncol], in_=tmp[tb][:, 0:ncol], func=AF.Exp,
                                                      bias=Fp[:, kc, h:h + 1], scale=1.0),
                        reads=(s_tmp[tb], s_Fp), writes=(s_P[pi],))
                if kc >= 4 * j:
                    self.op("dve", lambda E: E.tensor_tensor(out=P[pi][:, 0:128], in0=P[pi][:, 0:128],
                                                             in1=self.tribf[:], op=ALU.mult),
                            reads=(self.s_const,), writes=(s_P[pi],))

            def emit_PV(i):
                j, kc, nk = tiles[i]
                q0, ncol, pi = meta.pop(i)
                bO, bD = 5, 6
                self.op("pe", lambda E: E.matmul(self.pb[bO][:, q0:512], V[:, kc, h * 128:(h + 1) * 128],
                                                 P[pi][:, 0:ncol], start=(kc == 0), stop=(kc == nk - 1)),
                        reads=(s_V, s_P[pi]), writes=(self.pbs[bO],), signal=False)
                self.op("pe", lambda E: E.matmul(self.pb[bD][:, q0:512], self.onesbf[:], P[pi][:, 0:ncol],
                                                 start=(kc == 0), stop=(kc == nk - 1)),
                        reads=(self.s_const, s_P[pi]), writes=(self.pbs[bD],), signal=True)
                if kc == nk - 1:
                    self.op("dve", lambda E: E.reciprocal(out=rden[:], in_=self.pb[bD][:]), reads=(self.pbs[bD],),
                            writes=(s_rden,))
                    self.op("dve", lambda E: E.tensor_tensor(out=attn_out[:, h, j * 512:(j + 1) * 512], in0=self.pb[bO][:],
                                                             in1=rden[:], op=ALU.mult),
                            reads=(self.pbs[bO], s_rden), writes=(s_ao,))

            n = len(tiles)
            for i in range(n + LA):
                if i < n:
                    emit_S(i)
                if i - LA >= 0:
                    emit_PV(i - LA)

    def phase_merge(self, ph, l, hT, s_hT, pool_out, s_po, attn_out, s_ao):
        wsl = [self.sb(ph, f"wmg{i}", [128, 48, 128], BF16) for i in range(3)]
        s_w = [Slot() for _ in range(3)]
        bg = self.sb(ph, "bg", [128, 32], F32)
        s_bg = Slot()
        self.dma("sp", bg[:], self.b_gate[l:l + 1, :].rearrange("o (c p) -> p (o c)", p=128), writes=(s_bg,),
                 allow_slow_non_contiguous=True)
        sg = [self.sb(ph, f"sg{i}", [128, 512], F32) for i in range(4)]
        s_sg = [Slot() for _ in range(4)]
        mt = [self.sb(ph, f"mt{i}", [128, S], BF16) for i in range(2)]
        s_mt = [Slot(), Slot()]
        it = 0
        for c in range(KC):
            r = c % 3
            cs = slice(c * 128, (c + 1) * 128)
            self.dma("pool", wsl[r][:, 0:8, :], self.w_branch[l, 0, :, cs].rearrange("(k p) n -> p k n", p=128),
                     writes=(s_w[r],))
            self.dma("pool", wsl[r][:, 8:16, :], self.w_branch[l, 1, :, cs].rearrange("(k p) n -> p k n", p=128),
                     writes=(s_w[r],))
            self.dma("pool", wsl[r][:, 16:32, :], self.w_gate[l, :, cs].rearrange("(k p) n -> p k n", p=128),
                     writes=(s_w[r],))
            self.dma("pool", wsl[r][:, 32:48, :],
                     self.w_gate[l, :, D + c * 128:D + (c + 1) * 128].rearrange("(k p) n -> p k n", p=128),
                     writes=(s_w[r],))
            m = c % 2
            for tt in range(4):
                ts = slice(tt * 512, (tt + 1) * 512)
                b0 = 4 * (it % 2)
                so = 2 * (it % 2)
                it += 1
                for k in range(8):
                    self.op("pe", lambda E: E.matmul(self.pb[b0][:], wsl[r][:, k, :], pool_out[:, k, ts],
                                                     start=(k == 0), stop=(k == 7)),
                            reads=(s_w[r], s_po), writes=(self.pbs[b0],), signal=(k == 7))
                for k in range(8):
                    self.op("pe", lambda E: E.matmul(self.pb[b0 + 1][:], wsl[r][:, 8 + k, :], attn_out[:, k, ts],
                                                     start=(k == 0), stop=(k == 7)),
                            reads=(s_w[r], s_ao), writes=(self.pbs[b0 + 1],), signal=(k == 7))
                for gi in range(2):
                    for k in range(KC):
                        self.op("pe", lambda E: E.matmul(self.pb[b0 + 2 + gi][:], wsl[r][:, 16 + 16 * gi + k, :], hT[:, k, ts],
                                                         start=(k == 0), stop=(k == KC - 1)),
                                reads=(s_w[r], s_hT), writes=(self.pbs[b0 + 2 + gi],), signal=(k == KC - 1))
                for gi in range(2):
                    self.op("act", lambda E: E.activation(out=sg[so + gi][:], in_=self.pb[b0 + 2 + gi][:], func=AF.Sigmoid,
                                                          bias=bg[:, 16 * gi + c:16 * gi + c + 1], scale=1.0),
                            reads=(self.pbs[b0 + 2 + gi], s_bg), writes=(s_sg[so + gi],))
                for gi in range(2):
                    self.op("dve", lambda E: E.tensor_tensor(out=sg[so + gi][:], in0=self.pb[b0 + gi][:], in1=sg[so + gi][:],
                                                             op=ALU.mult),
                            reads=(self.pbs[b0 + gi],), writes=(s_sg[so + gi],))
                self.op("dve", lambda E: E.tensor_tensor(out=mt[m][:, ts], in0=sg[so][:], in1=sg[so + 1][:], op=ALU.add),
                        reads=(s_sg[so], s_sg[so + 1]), writes=(s_mt[m],))
            self.dma("sp", self.mT_d[:, :, c, :].rearrange("t p tok -> p t tok"), mt[m][:].rearrange("p (t tok) -> p t tok", tok=128), reads=(s_mt[m],))

    def phase_wout(self, ph, l, x_src):
        wo = self.sb(ph, "wo", [128, KC, D], BF16)
        s_wo = Slot()
        for q in range(8):
            self.dma("pool", wo[:, 2 * q:2 * q + 2, :],
                     self.w_out[l, q * 256:(q + 1) * 256, :].rearrange("(k p) n -> p k n", p=128), writes=(s_wo,))
        G = self.sb(ph, "G", [128, D], F32)
        s_G = Slot()
        self.dma("sp", G[:], self.mod_d[l:l + 1, 2 * D:3 * D].partition_broadcast(128), writes=(s_G,))
        mt = [self.sb(ph, f"mtl{i}", [128, KC, 128], BF16) for i in range(3)]
        s_mt = [Slot() for _ in range(3)]
        xt = [self.sb(ph, f"xw{i}", [128, D], F32) for i in range(2)]
        s_xt = [Slot(), Slot()]
        t1 = [self.sb(ph, f"t1w{i}", [128, 512], F32) for i in range(2)]
        s_t1 = [Slot(), Slot()]
        xn = [self.sb(ph, f"xn{i}", [128, D], F32) for i in range(2)]
        s_xn = [Slot(), Slot()]

        def load(t):
            self.dma("sp", mt[t % 3][:], self.mT_d[t], writes=(s_mt[t % 3],))
            self.dma("sp", xt[t % 2][:], x_src[t * 128:(t + 1) * 128, :], writes=(s_xt[t % 2],))

        load(0)
        i1 = 0
        for t in range(NT):
            if t + 1 < NT:
                load(t + 1)
            b = t % 2
            for k in range(KC):
                for n in range(4):
                    bk = 4 * (t % 2) + n
                    self.op("pe", lambda E: E.matmul(self.pb[bk][:], mt[t % 3][:, k, :], wo[:, k, n * 512:(n + 1) * 512],
                                                     start=(k == 0), stop=(k == KC - 1)),
                            reads=(s_mt[t % 3], s_wo), writes=(self.pbs[bk],), signal=(k == KC - 1))
            for n in range(4):
                bk = 4 * (t % 2) + n
                ns = slice(n * 512, (n + 1) * 512)
                q = i1 % 2
                i1 += 1
                self.op("dve", lambda E: E.tensor_tensor(out=t1[q][:], in0=self.pb[bk][:], in1=G[:, ns], op=ALU.mult),
                        reads=(self.pbs[bk], s_G), writes=(s_t1[q],))
                self.op("dve", lambda E: E.tensor_tensor(out=xn[b][:, ns], in0=t1[q][:], in1=xt[b][:, ns], op=ALU.add),
                        reads=(s_t1[q], s_xt[b]), writes=(s_xn[b],))
            self.dma("sp", self.xres[t * 128:(t + 1) * 128, :], xn[b][:], reads=(s_xn[b],))

    def layer_moe(self, l):
        if SPARSE:
            return self.layer_moe_sparse(l)
        with contextlib.ExitStack() as ph:
            hT = self.sb(ph, "h2T", [128, KC, S], BF16)
            s_hT = Slot()
            wr32 = self.sb(ph, "wr32", [128, KC, NE], F32)
            wr = self.sb(ph, "wr", [128, 2, KC, NE], BF16)
            wtmp = self.sb(ph, "wtmp", [128, KC, NE], F32)
            s_wr = Slot()
            for k in range(KC):
                self.dma("sp", wr32[:, k, :], self.w_router[k * 128:(k + 1) * 128, :], writes=(s_wr,))
            self.op("dve", lambda E: E.tensor_copy(wr[:, 0], wr32[:]), writes=(s_wr,))
            self.op("dve", lambda E: E.tensor_tensor(out=wtmp[:], in0=wr32[:], in1=wr[:, 0], op=ALU.subtract), writes=(s_wr,))
            self.op("dve", lambda E: E.tensor_copy(wr[:, 1], wtmp[:]), writes=(s_wr,))
            lg = self.sb(ph, "lg", [128, NT, NE], F32)
            s_lg = Slot()
            with contextlib.ExitStack() as ph2:
                import os
                self.phase_norm(ph2, l, self.xres, self.norm_moe[l:l + 1, :], 3 * D, 4 * D, hT, s_hT,
                                router=(None if os.environ.get("NOROUTER") == "1" else (wr, s_wr, lg, s_lg)))
            self.barrier()
            for q in range(4):
                self.dma("sp", self.h2T_d[:, q * 4:(q + 1) * 4, :], hT[:, q * 4:(q + 1) * 4, :], reads=(s_hT,))
            import os
            if os.environ.get('SKIPGATE') != '1':
                self.phase_gating(ph, lg, s_lg)
        self.barrier()
        if self.stop_after == f"gate{l}":
            return
        for half in range(2):
            with contextlib.ExitStack() as ph:
                self.phase_experts(ph, l, half)
            self.barrier()

    def phase_gating(self, ph, lg, s_lg, sparse=None):
        def T(name, shape):
            return self.sb(ph, name, shape, F32)
        s = s_lg
        mx = T("mx", [128, NT])
        sm = T("sm", [128, NT])
        pr = T("pr", [128, NT, NE])
        sel = T("sel", [128, NT, NE])
        brb = T("brb", [128, NE])
        psum6 = T("psum6", [128, NT, 4, 6])
        ind = T("ind", [128, NT, 4, 6])
        gmx = T("gmx", [128, NT])
        mask = T("mask", [128, NT, NE])
        gw = T("gw", [128, NT, NE])
        gs = T("gs", [128, NT])
        gwT = T("gwT", [NE, S])
        s_b = Slot()
        s_gwT = Slot()
        self.dma("sp", brb[:], self.b_router.partition_broadcast(128), writes=(s_b,))

        def dve(fn, extra_reads=()):
            return self.op("dve", fn, reads=extra_reads, writes=(s,))

        def bc(ap2):
            return ap2.unsqueeze(2).to_broadcast([128, NT, NE])

        dve(lambda E: E.tensor_reduce(out=mx[:], in_=lg[:], axis=AX.X, op=ALU.max))
        dve(lambda E: E.tensor_tensor(out=pr[:], in0=lg[:], in1=bc(mx[:]), op=ALU.subtract))
        self.op("act", lambda E: E.activation(out=pr[:], in_=pr[:], func=AF.Exp), writes=(s,))
        dve(lambda E: E.tensor_reduce(out=sm[:], in_=pr[:], axis=AX.X, op=ALU.add))
        dve(lambda E: E.reciprocal(out=sm[:], in_=sm[:]))
        dve(lambda E: E.tensor_tensor(out=pr[:], in0=pr[:], in1=bc(sm[:]), op=ALU.mult))
        dve(lambda E: E.tensor_tensor(out=sel[:], in0=pr[:], in1=brb[:].unsqueeze(1).to_broadcast([128, NT, NE]),
                                      op=ALU.add), extra_reads=(s_b,))
        sel4 = sel[:].rearrange("p t (g i) -> p t g i", i=4)
        pairs = [(0, 1), (0, 2), (0, 3), (1, 2), (1, 3), (2, 3)]
        for pi, (a, b) in enumerate(pairs):
            dve(lambda E: E.tensor_tensor(out=psum6[:, :, :, pi], in0=sel4[:, :, :, a], in1=sel4[:, :, :, b], op=ALU.add))
        dve(lambda E: E.tensor_reduce(out=gmx[:], in_=psum6[:].rearrange("p t g s -> p t (g s)"), axis=AX.X, op=ALU.max))
        dve(lambda E: E.tensor_tensor(out=ind[:].rearrange("p t g s -> p t (g s)"),
                                      in0=psum6[:].rearrange("p t g s -> p t (g s)"),
                                      in1=gmx[:].unsqueeze(2).to_broadcast([128, NT, 24]), op=ALU.is_equal))
        mask4 = mask[:].rearrange("p t (g i) -> p t g i", i=4)
        member = {0: (0, 1, 2), 1: (0, 3, 4), 2: (1, 3, 5), 3: (2, 4, 5)}
        for i in range(4):
            p0, p1, p2 = member[i]
            dve(lambda E: E.tensor_tensor(out=mask4[:, :, :, i], in0=ind[:, :, :, p0], in1=ind[:, :, :, p1], op=ALU.add))
            dve(lambda E: E.tensor_tensor(out=mask4[:, :, :, i], in0=mask4[:, :, :, i], in1=ind[:, :, :, p2], op=ALU.add))
        dve(lambda E: E.tensor_tensor(out=gw[:], in0=pr[:], in1=mask[:], op=ALU.mult))
        dve(lambda E: E.tensor_reduce(out=gs[:], in_=gw[:], axis=AX.X, op=ALU.add))
        dve(lambda E: E.reciprocal(out=gs[:], in_=gs[:]))
        dve(lambda E: E.tensor_tensor(out=gw[:], in0=gw[:], in1=bc(gs[:]), op=ALU.mult))
        if sparse is not None:
            return self.phase_positions(ph, sparse, s, mask, gw, dve, T)
        for t in range(NT):
            bk = (t // 4) % 2
            self.op("pe", lambda E: E.transpose(self.pb[bk][0:NE, (t % 4) * 128:(t % 4 + 1) * 128], gw[:, t, :],
                                                self.ident32[:]),
                    reads=(s, self.s_const), writes=(self.pbs[bk],), signal=True)
            if t % 4 == 3:
                self.op("act", lambda E: E.activation(out=gwT[:, (t - 3) * 128:(t + 1) * 128], in_=self.pb[bk][0:NE, :],
                                                      func=AF.Copy), reads=(self.pbs[bk],), writes=(s_gwT,))
        self.dma("sp", self.gwT_d, gwT[:], reads=(s_gwT,))

    def phase_experts(self, ph, l, half):
        acc = self.sb(ph, "acc", [128, 8, D], F32)
        s_acc = Slot()
        self._experts_inner(l, half, acc, s_acc)
        self.barrier()
        G2 = self.sb(ph, "G2", [128, D], F32)
        s_G2 = Slot()
        self.dma("sp", G2[:], self.mod_d[l:l + 1, 5 * D:6 * D].partition_broadcast(128), writes=(s_G2,))
        xt = [self.sb(ph, f"xm{i}", [128, D], F32) for i in range(2)]
        s_xt = [Slot(), Slot()]
        for sub in range(8):
            t = half * 8 + sub
            b = sub % 2
            self.dma("sp", xt[b][:], self.xres[t * 128:(t + 1) * 128, :], writes=(s_xt[b],))
            self.op("dve", lambda E: E.tensor_tensor(out=acc[:, sub, :], in0=acc[:, sub, :], in1=G2[:], op=ALU.mult),
                    reads=(s_G2,), writes=(s_acc,))
            self.op("dve", lambda E: E.tensor_tensor(out=xt[b][:], in0=acc[:, sub, :], in1=xt[b][:], op=ALU.add),
                    reads=(s_acc,), writes=(s_xt[b],))
            self.dma("sp", self.xres[t * 128:(t + 1) * 128, :], xt[b][:], reads=(s_xt[b],))

    def _experts_inner(self, l, half, acc, s_acc):
      with contextlib.ExitStack() as ph:
        HS = S // 2
        t0 = half * HS
        h2 = self.sb(ph, "h2", [128, KC, HS], BF16)
        s_h2 = Slot()
        for q in range(4):
            self.dma("sp", h2[:, q * 4:(q + 1) * 4, :], self.h2T_d[:, q * 4:(q + 1) * 4, t0:t0 + HS], writes=(s_h2,))
        actp = self.sb(ph, "actp", [128, 8, HS], BF16)
        s_actp = Slot()
        wd = self.sb(ph, "wd", [128, 8, D], BF16)
        s_wd = Slot()
        wgu = [self.sb(ph, f"wgu{i}", [128, 2, KC, 128], BF16) for i in range(3)]
        s_wgu = [Slot() for _ in range(3)]
        gwb = [self.sb(ph, f"gwb{i}", [128, HS], F32) for i in range(2)]
        s_gwb = [Slot(), Slot()]
        sa = [self.sb(ph, f"sa{i}", [128, 512], F32) for i in range(2)]
        s_sa = [Slot(), Slot()]
        iw = 0
        ia = 0
        for e in range(self.nexp):
            ge = e % 2
            self.dma("sp", gwb[ge][:], self.gwT_d[e:e + 1, t0:t0 + HS].partition_broadcast(128), writes=(s_gwb[ge],))
            for j in range(8):
                r = iw % 3
                iw += 1
                cs = slice(j * 128, (j + 1) * 128)
                self.dma("pool", wgu[r][:, 0], self.w_eg[l, e, :, cs].rearrange("(k p) n -> p k n", p=128),
                         writes=(s_wgu[r],))
                self.dma("pool", wgu[r][:, 1], self.w_eu[l, e, :, cs].rearrange("(k p) n -> p k n", p=128),
                         writes=(s_wgu[r],))
                for tt in range(2):
                    ts = slice(tt * 512, (tt + 1) * 512)
                    b0 = 2 * (ia % 2)
                    q = ia % 2
                    ia += 1
                    for which in range(2):
                        for k in range(KC):
                            self.op("pe", lambda E: E.matmul(self.pb[b0 + which][:], wgu[r][:, which, k, :], h2[:, k, ts],
                                                             start=(k == 0), stop=(k == KC - 1)),
                                    reads=(s_wgu[r], s_h2), writes=(self.pbs[b0 + which],), signal=(k == KC - 1))
                    self.op("act", lambda E: E.activation(out=sa[q][:], in_=self.pb[b0][:], func=AF.Silu),
                            reads=(self.pbs[b0],), writes=(s_sa[q],))
                    self.op("dve", lambda E: E.tensor_tensor(out=sa[q][:], in0=self.pb[b0 + 1][:], in1=sa[q][:], op=ALU.mult),
                            reads=(self.pbs[b0 + 1],), writes=(s_sa[q],))
                    self.op("dve", lambda E: E.tensor_tensor(out=actp[:, j, ts], in0=sa[q][:], in1=gwb[ge][:, ts], op=ALU.mult),
                            reads=(s_gwb[ge],), writes=(s_actp, s_sa[q]))
            for q in range(4):
                self.dma("pool", wd[:, 2 * q:2 * q + 2, :],
                         self.w_ed[l, e, q * 256:(q + 1) * 256, :].rearrange("(k p) n -> p k n", p=128), writes=(s_wd,))
            for sub in range(8):
                for n in range(4):
                    bk = 4 + n
                    for k in range(8):
                        self.op("pe", lambda E: E.matmul(self.pb[bk][:], actp[:, k, sub * 128:(sub + 1) * 128],
                                                         wd[:, k, n * 512:(n + 1) * 512], start=(k == 0), stop=(k == 7)),
                                reads=(s_actp, s_wd), writes=(self.pbs[bk],), signal=(k == 7))
                    ns = slice(n * 512, (n + 1) * 512)
                    if e == 0:
                        self.op("dve", lambda E: E.tensor_copy(acc[:, sub, ns], self.pb[bk][:]), reads=(self.pbs[bk],),
                                writes=(s_acc,))
                    else:
                        self.op("dve", lambda E: E.tensor_tensor(out=acc[:, sub, ns], in0=self.pb[bk][:],
                                                                 in1=acc[:, sub, ns], op=ALU.add),
                                reads=(self.pbs[bk],), writes=(s_acc,))

    def idma(self, out, in_, idx_ap, gather, reads=(), writes=(), extra=(), **kw):
        self._wait("pool", self._deps(reads, writes, extra))
        i = self._next_dsem("pool")
        self.dcnt[i] += 1
        off = bass.IndirectOffsetOnAxis(ap=idx_ap, axis=0)
        if gather:
            ins = self.nc.gpsimd.indirect_dma_start(out=out, out_offset=None, in_=in_, in_offset=off, **kw)
        else:
            ins = self.nc.gpsimd.indirect_dma_start(out=out, out_offset=off, in_=in_, in_offset=None, **kw)
        ins.then_inc(self.semh[f"d{i}"], 16)
        tok = (f"d{i}", 16 * self.dcnt[i])
        for sl in reads:
            if sl.r.get(tok[0], 0) < tok[1]:
                sl.r[tok[0]] = tok[1]
        for sl in writes:
            sl.w = tok
            sl.r = {}
        return tok

    def phase_positions(self, ph, sp, s, mask, gw, dve, T):
        maskb = self.sb(ph, "maskb", [128, NT * NE], BF16)
        rank = T("rank", [128, NT, NE])
        cnt = T("cnt", [128, NT, NE])
        toff = T("toff", [128, NT, NE])
        total = T("total", [128, NE])
        thr = T("thr", [128, NE, 16])
        cmp = T("cmp", [128, NE, 16])
        nblk = T("nblk", [128, NE])
        pend = T("pend", [128, NE])
        pstart = T("pstart", [128, NE])
        dest = T("dest", [128, NT, NE])
        destm = T("destm", [128, NT, NE])
        tmpm = T("tmpm", [128, NT, NE])
        dlo = T("dlo", [128, NT])
        dhi = T("dhi", [128, NT])
        bvals = T("bvals", [128, NBLK, NE])
        cmpb = T("cmpb", [128, NBLK, NE])
        be = T("be", [128, NBLK])
        prev = T("prev", [128, NBLK])
        chg = T("chg", [128, NBLK])
        t2 = T("t2", [128, NBLK])
        idxf = T("idxf", [128, NBLK])
        pcol = T("pcol", [128, 1])
        maskf = mask[:].rearrange("p t e -> p (t e)")
        dve(lambda E: E.tensor_copy(maskb[:], maskf))
        self.op("pe", lambda E: E.matmul(self.pb[0][:, 0:NT * NE], self.ustrbf[:], maskb[:], start=True, stop=True),
                reads=(s, self.s_const), writes=(self.pbs[0],))
        self.op("pe", lambda E: E.matmul(self.pb[1][:, 0:NT * NE], self.onesbf[:], maskb[:], start=True, stop=True),
                reads=(s, self.s_const), writes=(self.pbs[1],))
        self.op("pe", lambda E: E.matmul(self.pb[2][:, 0:1], self.ustrbf[:], self.onesbf[:, 0:1], start=True, stop=True),
                reads=(self.s_const,), writes=(self.pbs[2],))
        dve(lambda E: E.tensor_copy(rank[:].rearrange("p t e -> p (t e)"), self.pb[0][:, 0:NT * NE]), (self.pbs[0],))
        dve(lambda E: E.tensor_copy(cnt[:].rearrange("p t e -> p (t e)"), self.pb[1][:, 0:NT * NE]), (self.pbs[1],))
        dve(lambda E: E.tensor_copy(pcol[:], self.pb[2][:, 0:1]), (self.pbs[2],))
        dve(lambda E: E.memset(toff[:, 0, :], 0.0))
        for t in range(1, NT):
            dve(lambda E: E.tensor_tensor(out=toff[:, t, :], in0=toff[:, t - 1, :], in1=cnt[:, t - 1, :], op=ALU.add))
        dve(lambda E: E.tensor_tensor(out=total[:], in0=toff[:, NT - 1, :], in1=cnt[:, NT - 1, :], op=ALU.add))
        for m in range(16):
            dve(lambda E: E.memset(thr[:, :, m], 128.0 * m))
        dve(lambda E: E.tensor_tensor(out=cmp[:], in0=total[:].unsqueeze(2).to_broadcast([128, NE, 16]), in1=thr[:],
                                      op=ALU.is_gt))
        dve(lambda E: E.tensor_reduce(out=nblk[:], in_=cmp[:], axis=AX.X, op=ALU.add))
        dve(lambda E: E.tensor_copy(pend[:, 0:1], nblk[:, 0:1]))
        for e in range(1, NE):
            dve(lambda E: E.tensor_tensor(out=pend[:, e:e + 1], in0=pend[:, e - 1:e], in1=nblk[:, e:e + 1], op=ALU.add))
        dve(lambda E: E.tensor_tensor(out=pstart[:], in0=pend[:], in1=nblk[:], op=ALU.subtract))
        dve(lambda E: E.tensor_scalar(out=pstart[:], in0=pstart[:], scalar1=128.0, scalar2=None, op0=ALU.mult))
        dve(lambda E: E.tensor_tensor(out=dest[:], in0=rank[:], in1=toff[:], op=ALU.add))
        dve(lambda E: E.tensor_tensor(out=dest[:], in0=dest[:], in1=pstart[:].unsqueeze(1).to_broadcast([128, NT, NE]),
                                      op=ALU.add))
        dve(lambda E: E.tensor_scalar(out=tmpm[:], in0=mask[:], scalar1=-BIGIDX, scalar2=BIGIDX, op0=ALU.mult, op1=ALU.add))
        dve(lambda E: E.tensor_tensor(out=destm[:], in0=dest[:], in1=tmpm[:], op=ALU.add))
        dve(lambda E: E.tensor_reduce(out=dlo[:], in_=destm[:], axis=AX.X, op=ALU.min))
        dve(lambda E: E.tensor_tensor(out=tmpm[:], in0=dest[:], in1=mask[:], op=ALU.mult))
        dve(lambda E: E.tensor_reduce(out=dhi[:], in_=tmpm[:], axis=AX.X, op=ALU.max))
        dve(lambda E: E.tensor_tensor(out=tmpm[:], in0=destm[:], in1=dlo[:].unsqueeze(2).to_broadcast([128, NT, NE]),
                                      op=ALU.is_equal))
        dve(lambda E: E.tensor_tensor(out=tmpm[:], in0=tmpm[:], in1=gw[:], op=ALU.mult))
        dve(lambda E: E.tensor_reduce(out=sp["wlo"][:], in_=tmpm[:], axis=AX.X, op=ALU.add))
        dve(lambda E: E.tensor_scalar(out=sp["whi"][:], in0=sp["wlo"][:], scalar1=-1.0, scalar2=1.0, op0=ALU.mult,
                                      op1=ALU.add))
        dve(lambda E: E.tensor_copy(sp["dlo_i"][:], dlo[:]))
        dve(lambda E: E.tensor_copy(sp["dhi_i"][:], dhi[:]))
        for b in range(NBLK):
            dve(lambda E: E.memset(bvals[:, b, :], float(b)))
        dve(lambda E: E.tensor_tensor(out=cmpb[:], in0=pend[:].unsqueeze(1).to_broadcast([128, NBLK, NE]), in1=bvals[:],
                                      op=ALU.is_le))
        dve(lambda E: E.tensor_reduce(out=be[:], in_=cmpb[:], axis=AX.X, op=ALU.add))
        dve(lambda E: E.tensor_scalar(out=be[:], in0=be[:], scalar1=float(NE - 1), scalar2=None, op0=ALU.min))
        dve(lambda E: E.memset(prev[:, 0:1], -1.0))
        dve(lambda E: E.tensor_copy(prev[:, 1:NBLK], be[:, 0:NBLK - 1]))
        dve(lambda E: E.tensor_tensor(out=chg[:], in0=be[:], in1=prev[:], op=ALU.not_equal))
        if not SKIP:
            dve(lambda E: E.memset(chg[:], 1.0))
        dve(lambda E: E.tensor_scalar(out=t2[:], in0=chg[:], scalar1=-BIGIDX, scalar2=BIGIDX, op0=ALU.mult, op1=ALU.add))
        for name, rows in (("idxW_i", 2048.0), ("idxD_i", 1024.0)):
            dve(lambda E: E.tensor_scalar(out=idxf[:], in0=be[:], scalar1=rows, scalar2=pcol[:, 0:1], op0=ALU.mult,
                                          op1=ALU.add))
            dve(lambda E: E.tensor_tensor(out=idxf[:], in0=idxf[:], in1=chg[:], op=ALU.mult))
            dve(lambda E: E.tensor_tensor(out=idxf[:], in0=idxf[:], in1=t2[:], op=ALU.add))
            dve(lambda E: E.tensor_copy(sp[name][:], idxf[:]))
        if self.dbg:
            dbgt = T("dbgt", [128, 4 * NT + 2 * NBLK])
            dve(lambda E: E.tensor_copy(dbgt[:, 0:NT], dlo[:]))
            dve(lambda E: E.tensor_copy(dbgt[:, NT:2 * NT], dhi[:]))
            dve(lambda E: E.tensor_copy(dbgt[:, 2 * NT:3 * NT], sp["wlo"][:]))
            dve(lambda E: E.tensor_copy(dbgt[:, 3 * NT:4 * NT], sp["whi"][:]))
            dve(lambda E: E.tensor_copy(dbgt[:, 4 * NT:4 * NT + NBLK], be[:]))
            dve(lambda E: E.tensor_copy(dbgt[:, 4 * NT + NBLK:4 * NT + 2 * NBLK], idxf[:]))
            self.dma("sp", self.dbg_idx, dbgt[:], reads=(s,))

    def layer_moe_sparse(self, l):
        with contextlib.ExitStack() as pl:
            sp = dict(
                wlo=self.sb(pl, "wlo", [128, NT], F32), whi=self.sb(pl, "whi", [128, NT], F32),
                dlo_i=self.sb(pl, "dlo_i", [128, NT], I32), dhi_i=self.sb(pl, "dhi_i", [128, NT], I32),
                idxW_i=self.sb(pl, "idxW_i", [128, NBLK], I32), idxD_i=self.sb(pl, "idxD_i", [128, NBLK], I32))
            s_sp = Slot()
            with contextlib.ExitStack() as ph:
                h2tm = self.sb(ph, "h2tm", [128, NT, D], BF16)
                s_h2tm = Slot()
                hT = self.sb(ph, "hhi", [128, KC, 128], BF16)
                s_hT = Slot()
                wr32 = self.sb(ph, "wr32", [128, KC, NE], F32)
                wr = self.sb(ph, "wr", [128, 2, KC, NE], BF16)
                wtmp = self.sb(ph, "wtmp", [128, KC, NE], F32)
                s_wr = Slot()
                for k in range(KC):
                    self.dma("sp", wr32[:, k, :], self.w_router[k * 128:(k + 1) * 128, :], writes=(s_wr,))
                self.op("dve", lambda E: E.tensor_copy(wr[:, 0], wr32[:]), writes=(s_wr,))
                self.op("dve", lambda E: E.tensor_tensor(out=wtmp[:], in0=wr32[:], in1=wr[:, 0], op=ALU.subtract),
                        writes=(s_wr,))
                self.op("dve", lambda E: E.tensor_copy(wr[:, 1], wtmp[:]), writes=(s_wr,))
                lg = self.sb(ph, "lg", [128, NT, NE], F32)
                s_lg = Slot()
                with contextlib.ExitStack() as ph2:
                    self.phase_norm(ph2, l, self.xres, self.norm_moe[l:l + 1, :], 3 * D, 4 * D, hT, s_hT,
                                    router=(wr, s_wr, lg, s_lg), tm_out=(h2tm, s_h2tm))
                self.barrier()
                with contextlib.ExitStack() as ph2:
                    self.phase_gating(ph2, lg, s_lg, sparse=sp)
                self.barrier()
                for t in range(NT):
                    self.idma(self.buf_d, h2tm[:, t, :], sp["dlo_i"][:, t:t + 1], gather=False, reads=(s_h2tm,))
                    self.idma(self.buf_d, h2tm[:, t, :], sp["dhi_i"][:, t:t + 1], gather=False, reads=(s_h2tm,))
            self.barrier()
            if self.stop_after == f"gate{l}":
                return
            with contextlib.ExitStack() as ph:
                self.phase_blocks(ph, l, sp)
            self.barrier()
            with contextlib.ExitStack() as ph:
                self.phase_combine(ph, l, sp, final=(l == self.depth - 1 and self.stop_after is None))
            self.barrier()

    def phase_blocks(self, ph, l, sp):
        wg = self.sb(ph, "wg", [128, 8, 2, DE], BF16)
        wu = self.sb(ph, "wu", [128, 8, 2, DE], BF16)
        wd = self.sb(ph, "wd", [128, 8, D], BF16)
        s_wg = [Slot() for _ in range(8)]
        s_wu = [Slot() for _ in range(8)]
        s_wd = [Slot() for _ in range(8)]
        tabg = self.w_eg.rearrange("l e (r two) c -> (l e r) (two c)", two=2)
        tabu = self.w_eu.rearrange("l e (r two) c -> (l e r) (two c)", two=2)
        tabd = self.w_ed.rearrange("l e r c -> (l e r) c")
        xb = [self.sb(ph, f"xb{i}", [128, D], BF16) for i in range(2)]
        s_xb = [Slot(), Slot()]
        xbT = [self.sb(ph, f"xbT{i}", [128, KC, 128], BF16) for i in range(2)]
        s_xbT = [Slot(), Slot()]
        sa = [self.sb(ph, f"sab{i}", [128, 512], F32) for i in range(2)]
        s_sa = [Slot(), Slot()]
        act = self.sb(ph, "actb", [128, DE], BF16)
        s_act = Slot()
        actT = self.sb(ph, "actT", [128, 8, 128], BF16)
        s_actT = Slot()
        ysb = [self.sb(ph, f"ysb{i}", [128, D], F32) for i in range(2)]
        s_ysb = [Slot(), Slot()]
        pv = [self.pb[i][:].bitcast(BF16) for i in range(8)]
        if SKIP:
            if not hasattr(self, "_bc_regs"):
                self._bc_regs = (self.nc.gpsimd.to_reg(NE * D - 1), self.nc.gpsimd.to_reg(NE * DE - 1))
            kw_g = dict(bounds_check=self._bc_regs[0], oob_is_err=False)
            kw_d = dict(bounds_check=self._bc_regs[1], oob_is_err=False)
        else:
            kw_g, kw_d = {}, {}

        def load_xb(b):
            self.dma("sp", xb[b % 2][:], self.buf_d[b * 128:(b + 1) * 128, :], writes=(s_xb[b % 2],))

        load_xb(0)
        for b in range(NBLK):
            r = b % 2
            if b + 1 < NBLK:
                load_xb(b + 1)
            for k in range(8):
                eo = (l * NE * DE + k * 128) * 2 * DE
                self.idma(wg[:, k].rearrange("p two c -> p (two c)"), tabg, sp["idxD_i"][:, b:b + 1], gather=True,
                          writes=(s_wg[k],), element_offset=eo, **kw_d)
                self.idma(wu[:, k].rearrange("p two c -> p (two c)"), tabu, sp["idxD_i"][:, b:b + 1], gather=True,
                          writes=(s_wu[k],), element_offset=eo, **kw_d)
            for k in range(8):
                eo = (l * NE * DE + k * 128) * D
                self.idma(wd[:, k, :], tabd, sp["idxD_i"][:, b:b + 1], gather=True, writes=(s_wd[k],),
                          element_offset=eo, **kw_d)
            for g in range(2):
                bk = 4 + g
                for j in range(8):
                    k = g * 8 + j
                    k2, par = k // 2, k % 2
                    self.op("pe", lambda E: E.transpose(pv[bk][:, j * 128:(j + 1) * 128],
                                                        xb[r][:, k2 * 256 + par:(k2 + 1) * 256:2],
                                                        self.identbf[:]),
                            reads=(s_xb[r], self.s_const), writes=(self.pbs[bk],), signal=(j == 7))
                src = pv[bk][:].rearrange("p (j t) -> p j t", j=8)
                if g == 0:
                    self.op("act", lambda E: E.activation(out=xbT[r][:, 0:8, :], in_=src, func=AF.Copy),
                            reads=(self.pbs[bk],), writes=(s_xbT[r],))
                else:
                    self.op("dve", lambda E: E.tensor_copy(xbT[r][:, 8:16, :], src),
                            reads=(self.pbs[bk],), writes=(s_xbT[r],))
            for k in range(KC):
                for which, (wt, sw) in enumerate(((wg, s_wg), (wu, s_wu))):
                    for n in range(2):
                        bk = which * 2 + n
                        self.op("pe", lambda E: E.matmul(self.pb[bk][:], xbT[r][:, k, :],
                                                         wt[:, k // 2, k % 2, n * 512:(n + 1) * 512],
                                                         start=(k == 0), stop=(k == KC - 1)),
                                reads=(s_xbT[r], sw[k // 2]), writes=(self.pbs[bk],),
                                signal=(k == KC - 1 or (which == 1 and n == 1)))
            for n in range(2):
                self.op("act", lambda E: E.activation(out=sa[n][:], in_=self.pb[n][:], func=AF.Silu),
                        reads=(self.pbs[n],), writes=(s_sa[n],))
                self.op("dve", lambda E: E.tensor_tensor(out=act[:, n * 512:(n + 1) * 512], in0=self.pb[2 + n][:],
                                                         in1=sa[n][:], op=ALU.mult),
                        reads=(self.pbs[2 + n], s_sa[n]), writes=(s_act,))
            for j in range(8):
                self.op("pe", lambda E: E.transpose(pv[6][:, j * 128:(j + 1) * 128], act[:, j * 128:(j + 1) * 128],
                                                    self.identbf[:]),
                        reads=(s_act, self.s_const), writes=(self.pbs[6],), signal=(j == 7))
            self.op("act", lambda E: E.activation(out=actT[:], in_=pv[6][:].rearrange("p (j t) -> p j t", j=8),
                                                  func=AF.Copy), reads=(self.pbs[6],), writes=(s_actT,))
            for k in range(8):
                for n in range(4):
                    bk = 4 + n
                    self.op("pe", lambda E: E.matmul(self.pb[bk][:], actT[:, k, :], wd[:, k, n * 512:(n + 1) * 512],
                                                     start=(k == 0), stop=(k == 7)),
                            reads=(s_actT, s_wd[k]), writes=(self.pbs[bk],), signal=(k == 7))
            for n in range(4):
                bk = 4 + n
                ns = slice(n * 512, (n + 1) * 512)
                if n % 2 == 0:
                    self.op("act", lambda E: E.activation(out=ysb[r][:, ns], in_=self.pb[bk][:], func=AF.Copy),
                            reads=(self.pbs[bk],), writes=(s_ysb[r],))
                else:
                    self.op("dve", lambda E: E.tensor_copy(ysb[r][:, ns], self.pb[bk][:]),
                            reads=(self.pbs[bk],), writes=(s_ysb[r],))
            self.dma("sp", self.ybuf_d[b * 128:(b + 1) * 128, :], ysb[r][:], reads=(s_ysb[r],))

    def phase_combine(self, ph, l, sp, final=False):
        G2 = self.sb(ph, "G2c", [128, D], F32)
        s_G2 = Slot()
        if final:
            Gf = self.sb(ph, "Gfc", [128, D], F32)
            s_Gf = Slot()
            self.dma("sp", Gf[:], self.norm_final.partition_broadcast(128), writes=(s_Gf,))
            junk = self.sb(ph, "junkc", [128, D], BF16)
            s_junk = Slot()
            ssf = [self.sb(ph, f"ssc{i}", [128, 1], F32) for i in range(2)]
            s_ssf = [Slot(), Slot()]
        self.dma("sp", G2[:], self.mod_d[l:l + 1, 5 * D:6 * D].partition_broadcast(128), writes=(s_G2,))
        ylo = [self.sb(ph, f"ylo{i}", [128, D], F32) for i in range(2)]
        yhi = [self.sb(ph, f"yhi{i}", [128, D], F32) for i in range(2)]
        xt = [self.sb(ph, f"xc{i}", [128, D], F32) for i in range(2)]
        s_lo, s_hi, s_xt = [Slot(), Slot()], [Slot(), Slot()], [Slot(), Slot()]
        for t in range(NT):
            b = t % 2
            self.idma(ylo[b][:], self.ybuf_d, sp["dlo_i"][:, t:t + 1], gather=True, writes=(s_lo[b],))
            self.idma(yhi[b][:], self.ybuf_d, sp["dhi_i"][:, t:t + 1], gather=True, writes=(s_hi[b],))
            self.dma("sp", xt[b][:], self.xres[t * 128:(t + 1) * 128, :], writes=(s_xt[b],))
            self.op("dve", lambda E: E.tensor_scalar(out=ylo[b][:], in0=ylo[b][:], scalar1=sp["wlo"][:, t:t + 1], scalar2=None,
                                                     op0=ALU.mult), writes=(s_lo[b],))
            self.op("dve", lambda E: E.scalar_tensor_tensor(out=ylo[b][:], in0=yhi[b][:], scalar=sp["whi"][:, t:t + 1],
                                                            in1=ylo[b][:], op0=ALU.mult, op1=ALU.add),
                    reads=(s_hi[b],), writes=(s_lo[b],))
            self.op("dve", lambda E: E.tensor_tensor(out=ylo[b][:], in0=ylo[b][:], in1=G2[:], op=ALU.mult),
                    reads=(s_G2,), writes=(s_lo[b],))
            self.op("dve", lambda E: E.tensor_tensor(out=xt[b][:], in0=ylo[b][:], in1=xt[b][:], op=ALU.add),
                    reads=(s_lo[b],), writes=(s_xt[b],))
            if not final:
                self.dma("sp", self.xres[t * 128:(t + 1) * 128, :], xt[b][:], reads=(s_xt[b],))
            else:
                self.op("act", lambda E: E.activation(out=junk[:], in_=xt[b][:], func=AF.Square, accum_out=ssf[b][:]),
                        reads=(s_xt[b],), writes=(s_junk, s_ssf[b]))
                self.op("dve", lambda E: E.tensor_scalar(out=ssf[b][:], in0=ssf[b][:], scalar1=1.0 / D, scalar2=EPS,
                                                         op0=ALU.mult, op1=ALU.add), writes=(s_ssf[b],))
                self.op("act", lambda E: E.activation(out=ssf[b][:], in_=ssf[b][:], func=AF.Sqrt), writes=(s_ssf[b],))
                self.op("dve", lambda E: E.reciprocal(out=ssf[b][:], in_=ssf[b][:]), writes=(s_ssf[b],))
                self.op("dve", lambda E: E.scalar_tensor_tensor(out=yhi[b][:], in0=xt[b][:], scalar=ssf[b][:, 0:1], in1=Gf[:],
                                                                op0=ALU.mult, op1=ALU.mult),
                        reads=(s_xt[b], s_ssf[b], s_Gf), writes=(s_hi[b],))
                self.dma("sp", self.out[t * 128:(t + 1) * 128, :], yhi[b][:], reads=(s_hi[b],))

    def phase_final(self):
        with contextlib.ExitStack() as ph:
            Gf = self.sb(ph, "Gf", [128, D], F32)
            s_G = Slot()
            self.dma("sp", Gf[:], self.norm_final.partition_broadcast(128), writes=(s_G,))
            xt = [self.sb(ph, f"xf{i}", [128, D], F32) for i in range(2)]
            s_xt = [Slot(), Slot()]
            yo = [self.sb(ph, f"yo{i}", [128, D], F32) for i in range(2)]
            s_yo = [Slot(), Slot()]
            junk = self.sb(ph, "junkf", [128, D], BF16)
            s_junk = Slot()
            ss = [self.sb(ph, f"ssf{i}", [128, 1], F32) for i in range(2)]
            s_ss = [Slot(), Slot()]
            self.dma("sp", xt[0][:], self.xres[0:128, :], writes=(s_xt[0],))
            for t in range(NT):
                b = t % 2
                if t + 1 < NT:
                    self.dma("sp", xt[1 - b][:], self.xres[(t + 1) * 128:(t + 2) * 128, :], writes=(s_xt[1 - b],))
                self.op("act", lambda E: E.activation(out=junk[:], in_=xt[b][:], func=AF.Square, accum_out=ss[b][:]),
                        reads=(s_xt[b],), writes=(s_junk, s_ss[b]))
                self.op("dve", lambda E: E.tensor_scalar(out=ss[b][:], in0=ss[b][:], scalar1=1.0 / D, scalar2=EPS,
                                                         op0=ALU.mult, op1=ALU.add), writes=(s_ss[b],))
                self.op("act", lambda E: E.activation(out=ss[b][:], in_=ss[b][:], func=AF.Sqrt), writes=(s_ss[b],))
                self.op("dve", lambda E: E.reciprocal(out=ss[b][:], in_=ss[b][:]), writes=(s_ss[b],))
                self.op("dve", lambda E: E.scalar_tensor_tensor(out=yo[b][:], in0=xt[b][:], scalar=ss[b][:, 0:1], in1=Gf[:],
                                                                op0=ALU.mult, op1=ALU.mult),
                        reads=(s_xt[b], s_ss[b], s_G), writes=(s_yo[b],))
                self.dma("sp", self.out[t * 128:(t + 1) * 128, :], yo[b][:], reads=(s_yo[b],))
        self.barrier()


_WNAMES = ["w_ada", "b_ada", "norm_mix", "norm_moe", "w_in", "b_forget", "w_pool", "pool_scale", "w_branch", "w_gate",
           "b_gate", "w_out", "w_router", "w_exp_gate", "w_exp_up", "w_exp_down"]


def make_in_maps(inputs, cores):
    f = lambda a: np.ascontiguousarray(np.asarray(a, dtype=np.float32))
    shared = {n: f(inputs[n]) for n in _WNAMES}
    shared["b_router"] = f(inputs["b_router"]).reshape(1, NE)
    shared["norm_final"] = f(inputs["norm_final"]).reshape(1, D)
    x = f(inputs["x"])
    c = f(inputs["c"])
    maps = []
    for b in cores:
        m = dict(shared)
        m["x"] = np.ascontiguousarray(x[b])
        m["c"] = np.ascontiguousarray(c[b:b + 1])
        maps.append(m)
    return maps


def kernel(**inputs):
    prog = Prog(depth=2)
    nc = prog.build()
    maps = make_in_maps(inputs, list(range(8)))
    res = run_bass_kernel_spmd(nc, maps, core_ids=list(range(8)))
    return np.stack([r["out"] for r in res.results], axis=0).astype(np.float32)
```

```python
import contextlib
import numpy as np
import concourse.bass as bass
import concourse.mybir as mybir
from concourse.bass_utils import run_bass_kernel_spmd

F32 = mybir.dt.float32
BF16 = mybir.dt.bfloat16
AF = mybir.ActivationFunctionType
ALU = mybir.AluOpType
AX = mybir.AxisListType

S = 2048
D = 2048
NT = 16
KC = 16
INW = 4104
NE = 16
DE = 1024
EPS = 1e-6
NDS = 40
NBLK = 48
I32 = mybir.dt.int32
BIGIDX = 1000000.0
SPARSE = True
SKIP = True


class Slot:
    __slots__ = ("w", "r")

    def __init__(self):
        self.w = None
        self.r = {}


class Prog:
    def __init__(self, depth=2, dbg=False, stop_after=None, nexp=NE):
        self.depth = depth
        self.dbg = dbg
        self.stop_after = stop_after
        self.nexp = nexp
        nc = bass.Bass("TRN2", target_bir_lowering=False)
        self.nc = nc
        self.es = contextlib.ExitStack()
        self.eng = dict(pe=nc.tensor, act=nc.scalar, dve=nc.vector, pool=nc.gpsimd, sp=nc.sync)
        self.semh = {}
        for e in self.eng:
            self.semh[e] = self.es.enter_context(nc.semaphore("p_" + e))
        for i in range(NDS):
            self.semh[f"d{i}"] = self.es.enter_context(nc.semaphore(f"dq{i}"))
        self.cnt = {e: 0 for e in self.eng}
        self.dcnt = [0] * NDS
        self.dnext = 0
        self._dq = {}
        self.seen = {e: {} for e in self.eng}
        self.pend = {e: ([], []) for e in self.eng}
        self.last = {}

    def _wait(self, e, deps):
        best = {}
        for d in deps:
            if d is not None:
                if best.get(d[0], 0) < d[1]:
                    best[d[0]] = d[1]
        for s, v in best.items():
            if self.seen[e].get(s, 0) < v:
                self.eng[e].wait_ge(self.semh[s], v)
                self.seen[e][s] = v

    def _deps(self, reads, writes, extra):
        deps = list(extra)
        for s in reads:
            deps.append(s.w)
        for s in writes:
            deps.append(s.w)
            deps.extend(s.r.items())
        return deps

    def op(self, e, build, reads=(), writes=(), extra=(), signal=True):
        self._wait(e, self._deps(reads, writes, extra))
        ins = build(self.eng[e])
        pr, pw = self.pend[e]
        pr.extend(reads)
        pw.extend(writes)
        if not signal:
            return None
        self.cnt[e] += 1
        ins.then_inc(self.semh[e], 1)
        tok = (e, self.cnt[e])
        for s in pr:
            if s.r.get(e, 0) < tok[1]:
                s.r[e] = tok[1]
        for s in pw:
            s.w = tok
            s.r = {}
        pr.clear()
        pw.clear()
        self.last[e] = tok
        return tok

    def dma(self, q, out, in_, reads=(), writes=(), extra=(), **kw):
        self._wait(q, self._deps(reads, writes, extra))
        i = self._next_dsem(q)
        self.dcnt[i] += 1
        self.eng[q].dma_start(out=out, in_=in_, **kw).then_inc(self.semh[f"d{i}"], 16)
        tok = (f"d{i}", 16 * self.dcnt[i])
        for s in reads:
            if s.r.get(tok[0], 0) < tok[1]:
                s.r[tok[0]] = tok[1]
        for s in writes:
            s.w = tok
            s.r = {}
        return tok

    def _next_dsem(self, q):
        lo, n = (0, 16) if q == "sp" else (16, NDS - 16)
        st = self._dq.setdefault(q, 0)
        self._dq[q] = (st + 1) % n
        return lo + st

    def barrier(self):
        toks = [t for t in self.last.values()]
        toks += [(f"d{i}", 16 * self.dcnt[i]) for i in range(NDS) if self.dcnt[i] > 0]
        for e in self.eng:
            self._wait(e, toks)

    def sb(self, ph, name, shape, dt):
        self._n = getattr(self, "_n", 0) + 1
        return ph.enter_context(self.nc.sbuf_tensor(f"{name}_{self._n}", shape, dt))

    def dram(self, name, shape, dt, kind):
        return self.nc.dram_tensor(name, shape, dt, kind=kind).ap()

    def build(self):
        nc = self.nc
        I = "ExternalInput"
        self.x = self.dram("x", [S, D], F32, I)
        self.c = self.dram("c", [1, D], F32, I)
        self.w_ada = self.dram("w_ada", [2, D, 6 * D], F32, I)
        self.b_ada = self.dram("b_ada", [2, 6 * D], F32, I)
        self.norm_mix = self.dram("norm_mix", [2, D], F32, I)
        self.norm_moe = self.dram("norm_moe", [2, D], F32, I)
        self.w_in = self.dram("w_in", [2, D, INW], F32, I)
        self.b_forget = self.dram("b_forget", [2, 8], F32, I)
        self.w_pool = self.dram("w_pool", [2, 4, 256, 256], F32, I)
        self.pool_scale = self.dram("pool_scale", [2, 1024], F32, I)
        self.w_branch = self.dram("w_branch", [2, 2, 1024, D], F32, I)
        self.w_gate = self.dram("w_gate", [2, D, 2 * D], F32, I)
        self.b_gate = self.dram("b_gate", [2, 2 * D], F32, I)
        self.w_out = self.dram("w_out", [2, D, D], F32, I)
        self.w_router = self.dram("w_router", [D, NE], F32, I)
        self.b_router = self.dram("b_router", [1, NE], F32, I)
        small = self.stop_after is not None and (self.stop_after == "mod" or self.stop_after.startswith("mixer") or self.stop_after.startswith("gate"))
        self.small_exp = small
        eshape = [2, NE, 1, 8] if small else None
        self.w_eg = self.dram("w_exp_gate", eshape or [2, NE, D, DE], F32, I)
        self.w_eu = self.dram("w_exp_up", eshape or [2, NE, D, DE], F32, I)
        self.w_ed = self.dram("w_exp_down", eshape or [2, NE, DE, D], F32, I)
        self.norm_final = self.dram("norm_final", [1, D], F32, I)
        self.out = self.dram("out", [S, D], F32, "ExternalOutput")
        sk = "ExternalOutput" if self.dbg else "Internal"
        self.xres = self.dram("xres", [S, D], F32, sk)
        self.mod_d = self.dram("mod_d", [2, 6 * D], F32, sk)
        self.h2T_d = self.dram("h2T_d", [128, KC, S], BF16, sk)
        self.mT_d = self.dram("mT_d", [NT, 128, KC, 128], BF16, sk)
        self.gwT_d = self.dram("gwT_d", [NE, S], F32, sk)
        self.buf_d = self.dram("buf_d", [NBLK * 128, D], BF16, "Internal")
        self.ybuf_d = self.dram("ybuf_d", [NBLK * 128, D], F32, "Internal")
        if self.dbg:
            self.dbg_idx = self.dram("dbg_idx", [128, 4 * NT + 2 * NBLK], F32, sk)
        if self.dbg:
            self.dbg_h = self.dram("dbg_h", [128, KC, S], BF16, sk)
            self.dbg_pool = self.dram("dbg_pool", [128, 8, S], BF16, sk)
            self.dbg_attn = self.dram("dbg_attn", [128, 8, S], BF16, sk)
            self.dbg_F = self.dram("dbg_F", [128, NT, 8], F32, sk)

        with self.es:
            es = self.es
            self.pb = [es.enter_context(nc.psum_tensor(f"pb{i}", [128, 512], F32)) for i in range(8)]
            self.pbs = [Slot() for _ in range(8)]
            self.ident32 = self.sb(es, "ident32", [128, 128], F32)
            self.ones32 = self.sb(es, "ones32", [128, 128], F32)
            self.uinc32 = self.sb(es, "uinc32", [128, 128], F32)
            self.onesbf = self.sb(es, "onesbf", [128, 128], BF16)
            self.tribf = self.sb(es, "tribf", [128, 128], BF16)
            self.s_const = Slot()
            g = nc.gpsimd
            self.op("pool", lambda E: E.memset(self.ident32[:], 1.0), writes=(self.s_const,))
            self.op("pool", lambda E: E.affine_select(out=self.ident32[:], in_=self.ident32[:], pattern=[[-1, 128]],
                                                      compare_op=ALU.is_equal, fill=0.0, base=0, channel_multiplier=1),
                    writes=(self.s_const,))
            self.op("pool", lambda E: E.memset(self.ones32[:], 1.0), writes=(self.s_const,))
            self.op("pool", lambda E: E.memset(self.onesbf[:], 1.0), writes=(self.s_const,))
            self.op("pool", lambda E: E.memset(self.uinc32[:], 1.0), writes=(self.s_const,))
            self.op("pool", lambda E: E.affine_select(out=self.uinc32[:], in_=self.uinc32[:], pattern=[[1, 128]],
                                                      compare_op=ALU.is_ge, fill=0.0, base=0, channel_multiplier=-1),
                    writes=(self.s_const,))
            self.op("pool", lambda E: E.tensor_copy(self.tribf[:], self.uinc32[:]), writes=(self.s_const,))
            self.identbf = self.sb(es, "identbf", [128, 128], BF16)
            self.op("pool", lambda E: E.tensor_copy(self.identbf[:], self.ident32[:]), writes=(self.s_const,))
            self.ustr32 = self.sb(es, "ustr32", [128, 128], F32)
            self.ustrbf = self.sb(es, "ustrbf", [128, 128], BF16)
            self.op("pool", lambda E: E.memset(self.ustr32[:], 1.0), writes=(self.s_const,))
            self.op("pool", lambda E: E.affine_select(out=self.ustr32[:], in_=self.ustr32[:], pattern=[[1, 128]],
                                                      compare_op=ALU.is_gt, fill=0.0, base=0, channel_multiplier=-1),
                    writes=(self.s_const,))
            self.op("pool", lambda E: E.tensor_copy(self.ustrbf[:], self.ustr32[:]), writes=(self.s_const,))
            self.barrier()

            self.phase_mod()
            if self.stop_after == "mod":
                return self._finish()
            for l in range(self.depth):
                x_src = self.x if l == 0 else self.xres
                self.layer_mixer(l, x_src)
                if self.stop_after == f"mixer{l}":
                    return self._finish()
                self.layer_moe(l)
                if self.stop_after in (f"moe{l}", f"gate{l}"):
                    return self._finish()
            self.phase_final()
            return self._finish()

    def _finish(self):
        self.barrier()
        return self.nc

    def phase_mod(self):
        nc = self.nc
        with contextlib.ExitStack() as ph:
            cT = self.sb(ph, "cT", [128, 16], F32)
            cact = self.sb(ph, "cact", [128, 16], BF16)
            s_c, s_ca, s_b, s_m = Slot(), Slot(), Slot(), Slot()
            self.dma("sp", cT[:], self.c.rearrange("o (k p) -> p (o k)", p=128), writes=(s_c,),
                     allow_slow_non_contiguous=True)
            self.op("act", lambda E: E.activation(out=cact[:], in_=cT[:], func=AF.Silu), reads=(s_c,), writes=(s_ca,))
            wring = [self.sb(ph, f"wada{i}", [128, 3072], BF16) for i in range(4)]
            wsl = [Slot() for _ in range(4)]
            brow = self.sb(ph, "brow", [1, 6 * D], F32)
            mrow = self.sb(ph, "mrow", [1, 6 * D], F32)
            i = 0
            for l in range(self.depth):
                self.dma("sp", brow[:], self.b_ada[l:l + 1, :], writes=(s_b,))
                for grp in range(4):
                    for k in range(16):
                        r = i % 4
                        i += 1
                        self.dma("pool", wring[r][:], self.w_ada[l, k * 128:(k + 1) * 128, grp * 3072:(grp + 1) * 3072],
                                 writes=(wsl[r],))
                        for n in range(6):
                            self.op("pe", lambda E: E.matmul(self.pb[n][0:1, :], cact[:, k:k + 1],
                                                             wring[r][:, n * 512:(n + 1) * 512],
                                                             start=(k == 0), stop=(k == 15)),
                                    reads=(s_ca, wsl[r]), writes=(self.pbs[n],), signal=(n == 5))
                    for n in range(6):
                        col = grp * 3072 + n * 512
                        self.op("dve", lambda E: E.tensor_tensor(out=mrow[0:1, col:col + 512], in0=self.pb[n][0:1, :],
                                                                 in1=brow[0:1, col:col + 512], op=ALU.add),
                                reads=(self.pbs[n], s_b), writes=(s_m,))
                self.dma("sp", self.mod_d[l:l + 1, :], mrow[:], reads=(s_m,))
        self.barrier()

    def phase_norm(self, ph, l, x_src, gnorm_row, off_shift, off_scale, hT, s_hT, router=None, tm_out=None):
        A = self.sb(ph, "A", [128, D], F32)
        B = self.sb(ph, "B", [128, D], F32)
        s_A, s_B = Slot(), Slot()
        self.dma("sp", A[:], self.mod_d[l:l + 1, off_scale:off_scale + D].partition_broadcast(128), writes=(s_A,))
        self.dma("sp", B[:], gnorm_row.partition_broadcast(128), writes=(s_B,))
        self.op("dve", lambda E: E.scalar_tensor_tensor(out=A[:], in0=A[:], scalar=1.0, in1=B[:], op0=ALU.add,
                                                        op1=ALU.mult), reads=(s_B,), writes=(s_A,))
        self.dma("sp", B[:], self.mod_d[l:l + 1, off_shift:off_shift + D].partition_broadcast(128), writes=(s_B,))
        xt = [self.sb(ph, f"xt{i}", [128, D], F32) for i in range(2)]
        s_xt = [Slot(), Slot()]
        junk = self.sb(ph, "junk", [128, D], BF16)
        s_junk = Slot()
        ss = [self.sb(ph, f"ss{i}", [128, 1], F32) for i in range(2)]
        s_ss = [Slot(), Slot()]
        t1 = [self.sb(ph, f"t1{i}", [128, D], F32) for i in range(2)]
        s_t1 = [Slot(), Slot()]
        if router is not None:
            hT32 = self.sb(ph, "hlo", [128, KC, 128], BF16)
            s_h32 = Slot()
            wr, s_wr, lg, s_lg = router
        ssall = self.sb(ph, "ssall", [128, NT], F32)
        s_ssall = Slot()

        def load(i):
            t = i % NT
            self.dma("sp", xt[i % 2][:], x_src[t * 128:(t + 1) * 128, :], writes=(s_xt[i % 2],))

        load(0)
        for t in range(NT):
            b = t % 2
            load(t + 1)
            self.op("act", lambda E: E.activation(out=junk[:], in_=xt[b][:], func=AF.Square, accum_out=ssall[:, t:t + 1]),
                    reads=(s_xt[b],), writes=(s_junk, s_ssall))
        self.op("dve", lambda E: E.tensor_scalar(out=ssall[:], in0=ssall[:], scalar1=1.0 / D, scalar2=EPS,
                                                 op0=ALU.mult, op1=ALU.add), writes=(s_ssall,))
        self.op("act", lambda E: E.activation(out=ssall[:], in_=ssall[:], func=AF.Sqrt), writes=(s_ssall,))
        self.op("dve", lambda E: E.reciprocal(out=ssall[:], in_=ssall[:]), writes=(s_ssall,))
        for t in range(NT):
            b = t % 2
            if t + 1 < NT:
                load(NT + t + 1)
            self.op("dve", lambda E: E.scalar_tensor_tensor(out=t1[b][:], in0=xt[b][:], scalar=ssall[:, t:t + 1], in1=A[:],
                                                            op0=ALU.mult, op1=ALU.mult),
                    reads=(s_xt[b], s_ssall, s_A), writes=(s_t1[b],))
            self.op("dve", lambda E: E.tensor_tensor(out=t1[b][:], in0=t1[b][:], in1=B[:], op=ALU.add),
                    reads=(s_B,), writes=(s_t1[b],))
            hc = slice(0, 128) if tm_out is not None else slice(t * 128, (t + 1) * 128)
            if tm_out is not None:
                h2tm, s_h2tm = tm_out
                self.op("pool", lambda E: E.tensor_copy(h2tm[:, t, :], t1[b][:]), reads=(s_t1[b],), writes=(s_h2tm,))
            for g in range(4):
                pbk = 2 * (t % 2) + (g % 2) if router is not None else (4 * (t % 2) + g)
                for j in range(4):
                    k = g * 4 + j
                    self.op("pe", lambda E: E.transpose(self.pb[pbk][:, j * 128:(j + 1) * 128],
                                                        t1[b][:, k * 128:(k + 1) * 128], self.ident32[:]),
                            reads=(s_t1[b], self.s_const), writes=(self.pbs[pbk],), signal=(j == 3))
                src = self.pb[pbk][:].rearrange("p (j t) -> p j t", j=4)
                self.op("act", lambda E: E.activation(out=hT[:, g * 4:(g + 1) * 4, hc], in_=src,
                                                      func=AF.Copy), reads=(self.pbs[pbk],), writes=(s_hT,))
                if router is not None:
                    self.op("dve", lambda E: E.tensor_tensor(out=hT32[:, g * 4:(g + 1) * 4, :], in0=src,
                                                             in1=hT[:, g * 4:(g + 1) * 4, hc],
                                                             op=ALU.subtract),
                            reads=(self.pbs[pbk], s_hT), writes=(s_h32,))
            if router is not None:
                rb = 4 + (t % 4)
                for k in range(KC):
                    hi = hT[:, k, hc]
                    lo = hT32[:, k, :]
                    for ii, (lh, rw) in enumerate(((hi, 0), (lo, 0), (hi, 1))):
                        self.op("pe", lambda E: E.matmul(self.pb[rb][:, 0:NE], lh, wr[:, rw, k, :],
                                                         start=(k == 0 and ii == 0), stop=(k == KC - 1 and ii == 2)),
                                reads=(s_h32, s_wr, s_hT), writes=(self.pbs[rb],), signal=(k == KC - 1 and ii == 2))
                self.op("dve", lambda E: E.tensor_copy(lg[:, t, :], self.pb[rb][:, 0:NE]),
                        reads=(self.pbs[rb],), writes=(s_lg,))

    def layer_mixer(self, l, x_src):
        nc = self.nc
        with contextlib.ExitStack() as ph:
            hT = self.sb(ph, "hT", [128, KC, S], BF16)
            s_hT = Slot()
            with contextlib.ExitStack() as ph2:
                self.phase_norm(ph2, l, x_src, self.norm_mix[l:l + 1, :], 0, D, hT, s_hT)
            self.barrier()
            if self.dbg and l == 0:
                self.dma("sp", self.dbg_h, hT[:], reads=(s_hT,))
            pool_out = self.sb(ph, "pool_out", [128, 8, S], BF16)
            attn_out = self.sb(ph, "attn_out", [128, 8, S], BF16)
            s_po, s_ao = Slot(), Slot()
            with contextlib.ExitStack() as ph2:
                self.phase_pool(ph2, l, hT, s_hT, pool_out, s_po)
            self.barrier()
            with contextlib.ExitStack() as ph2:
                self.phase_attn(ph2, l, hT, s_hT, attn_out, s_ao)
            self.barrier()
            if self.dbg and l == 0:
                self.dma("sp", self.dbg_pool, pool_out[:], reads=(s_po,))
                self.dma("sp", self.dbg_attn, attn_out[:], reads=(s_ao,))
            with contextlib.ExitStack() as ph2:
                self.phase_merge(ph2, l, hT, s_hT, pool_out, s_po, attn_out, s_ao)
            self.barrier()
        with contextlib.ExitStack() as ph:
            self.phase_wout(ph, l, x_src)
        self.barrier()

    def wblock(self, dst, src_cols):
        return src_cols.rearrange("(k p) n -> p k n", p=128)

    def phase_pool(self, ph, l, hT, s_hT, pool_out, s_po):
        PADW = 16
        pooled = self.sb(ph, "pooled", [128, 8, S], BF16)
        s_pl = Slot()
        wblk = [self.sb(ph, f"wblk{i}", [128, KC, 128], BF16) for i in range(3)]
        s_wb = [Slot() for _ in range(3)]
        u32 = self.sb(ph, "u32", [128, PADW + S], F32)
        sA = self.sb(ph, "sA", [128, PADW + S], F32)
        sB = self.sb(ph, "sB", [128, PADW + S], F32)
        s_u, s_sA, s_sB = Slot(), Slot(), Slot()
        invc = self.sb(ph, "invc", [128, 16], F32)
        tmpf = self.sb(ph, "tmpf", [128, 16], F32)
        s_inv, s_tmpf = Slot(), Slot()
        psc = self.sb(ph, "psc", [128, 8], F32)
        s_psc = Slot()
        wp = self.sb(ph, "wp", [128, 8, 256], BF16)
        s_wp = Slot()
        self.dma("sp", psc[:], self.pool_scale[l:l + 1, :].rearrange("o (c p) -> p (o c)", p=128), writes=(s_psc,),
                 allow_slow_non_contiguous=True)
        self.dma("pool", wp[:], self.w_pool[l].rearrange("g (i p) d -> p (g i) d", p=128), writes=(s_wp,))
        for buf, sl in ((u32, s_u), (sA, s_sA), (sB, s_sB)):
            self.op("dve", lambda E: E.memset(buf[:, 0:PADW], 0.0), writes=(sl,))
        for tcol in range(16):
            self.op("dve", lambda E: E.memset(invc[:, tcol:tcol + 1], 1.0 / (tcol + 1)), writes=(s_inv,))
        for c in range(8):
            r = c % 3
            self.dma("pool", wblk[r][:], self.w_in[l, :, c * 128:(c + 1) * 128].rearrange("(k p) n -> p k n", p=128),
                     writes=(s_wb[r],))
            for tt in range(4):
                bk = (c * 4 + tt) % 8
                for k in range(KC):
                    self.op("pe", lambda E: E.matmul(self.pb[bk][:], wblk[r][:, k, :], hT[:, k, tt * 512:(tt + 1) * 512],
                                                     start=(k == 0), stop=(k == KC - 1)),
                            reads=(s_wb[r], s_hT), writes=(self.pbs[bk],), signal=(k == KC - 1))
                self.op("act", lambda E: E.activation(out=u32[:, PADW + tt * 512:PADW + (tt + 1) * 512], in_=self.pb[bk][:],
                                                      func=AF.Copy), reads=(self.pbs[bk],), writes=(s_u,))
            g = c // 2
            w = 2 << g
            cur, s_cur = u32, s_u
            nxt = [(sA, s_sA), (sB, s_sB)]
            for i in range(g + 1):
                sh = 1 << i
                dst, s_dst = nxt[i % 2]
                self.op("dve", lambda E: E.tensor_tensor(out=dst[:, PADW:], in0=cur[:, PADW:],
                                                         in1=cur[:, PADW - sh:PADW - sh + S], op=ALU.add),
                        reads=(s_cur,), writes=(s_dst,))
                cur, s_cur = dst, s_dst
            self.op("dve", lambda E: E.scalar_tensor_tensor(out=pooled[:, c, :], in0=cur[:, PADW:], scalar=1.0 / w,
                                                            in1=u32[:, PADW:], op0=ALU.mult, op1=ALU.subtract),
                    reads=(s_cur, s_u), writes=(s_pl,))
            self.op("dve", lambda E: E.tensor_tensor(out=tmpf[:, 0:w], in0=cur[:, PADW:PADW + w], in1=invc[:, 0:w],
                                                     op=ALU.mult), reads=(s_cur, s_inv), writes=(s_tmpf,))
            self.op("dve", lambda E: E.tensor_tensor(out=pooled[:, c, 0:w], in0=tmpf[:, 0:w], in1=u32[:, PADW:PADW + w],
                                                     op=ALU.subtract), reads=(s_tmpf, s_u), writes=(s_pl,))
        for g in range(4):
            for oc in range(2):
                for tt in range(4):
                    bk = (g * 8 + oc * 4 + tt) % 8
                    for ic in range(2):
                        self.op("pe", lambda E: E.matmul(self.pb[bk][:], wp[:, g * 2 + ic, oc * 128:(oc + 1) * 128],
                                                         pooled[:, g * 2 + ic, tt * 512:(tt + 1) * 512],
                                                         start=(ic == 0), stop=(ic == 1)),
                                reads=(s_wp, s_pl), writes=(self.pbs[bk],), signal=(ic == 1))
                    cc = g * 2 + oc
                    self.op("act", lambda E: E.activation(out=pool_out[:, cc, tt * 512:(tt + 1) * 512], in_=self.pb[bk][:],
                                                          func=AF.Identity, scale=psc[:, cc:cc + 1]),
                            reads=(self.pbs[bk], s_psc), writes=(s_po,))

    def phase_attn(self, ph, l, hT, s_hT, attn_out, s_ao):
        scale = 128 ** -0.5
        V = self.sb(ph, "V", [128, NT, 1024], BF16)
        s_V = Slot()
        Fp = self.sb(ph, "Fp", [128, NT, 8], F32)
        s_Fp = Slot()
        with contextlib.ExitStack() as p3:
            wv = self.sb(p3, "wv", [128, KC, 1024], BF16)
            wf = self.sb(p3, "wf", [128, KC, 8], BF16)
            s_wv, s_wf = Slot(), Slot()
            for q in range(4):
                self.dma("pool", wv[:, q * 4:(q + 1) * 4, :],
                         self.w_in[l, q * 512:(q + 1) * 512, 3072:4096].rearrange("(k p) n -> p k n", p=128),
                         writes=(s_wv,))
            self.dma("pool", wf[:], self.w_in[l, :, 4096:4104].rearrange("(k p) n -> p k n", p=128), writes=(s_wf,))
            bfb = self.sb(p3, "bfb", [128, 8], F32)
            s_bfb = Slot()
            self.dma("sp", bfb[:], self.b_forget[l:l + 1, :].partition_broadcast(128), writes=(s_bfb,))
            z = self.sb(p3, "z", [128, NT, 8], F32)
            s_z = Slot()
            for t in range(NT):
                for k in range(KC):
                    for hh in range(2):
                        bk = (t * 3 + hh) % 8
                        self.op("pe", lambda E: E.matmul(self.pb[bk][:], hT[:, k, t * 128:(t + 1) * 128],
                                                         wv[:, k, hh * 512:(hh + 1) * 512],
                                                         start=(k == 0), stop=(k == KC - 1)),
                                reads=(s_hT, s_wv), writes=(self.pbs[bk],), signal=(k == KC - 1))
                for hh in range(2):
                    bk = (t * 3 + hh) % 8
                    if hh == 0:
                        self.op("act", lambda E: E.activation(out=V[:, t, hh * 512:(hh + 1) * 512], in_=self.pb[bk][:],
                                                              func=AF.Copy), reads=(self.pbs[bk],), writes=(s_V,))
                    else:
                        self.op("dve", lambda E: E.tensor_copy(V[:, t, hh * 512:(hh + 1) * 512], self.pb[bk][:]),
                                reads=(self.pbs[bk],), writes=(s_V,))
                bk = (t * 3 + 2) % 8
                for k in range(KC):
                    self.op("pe", lambda E: E.matmul(self.pb[bk][:, 0:8], hT[:, k, t * 128:(t + 1) * 128], wf[:, k, :],
                                                     start=(k == 0), stop=(k == KC - 1)),
                            reads=(s_hT, s_wf), writes=(self.pbs[bk],), signal=(k == KC - 1))
                self.op("dve", lambda E: E.tensor_tensor(out=z[:, t, :], in0=self.pb[bk][:, 0:8], in1=bfb[:], op=ALU.add),
                        reads=(self.pbs[bk], s_bfb), writes=(s_z,))
            zf = z[:].rearrange("p t h -> p (t h)")
            self.op("act", lambda E: E.activation(out=zf, in_=zf, func=AF.Exp, scale=-1.0), writes=(s_z,))
            self.op("act", lambda E: E.activation(out=zf, in_=zf, func=AF.Ln, bias=1.0, scale=1.0), writes=(s_z,))
            self.op("pe", lambda E: E.matmul(self.pb[0][:, 0:128], self.uinc32[:], zf, start=True, stop=True),
                    reads=(s_z, self.s_const), writes=(self.pbs[0],))
            self.op("pe", lambda E: E.matmul(self.pb[1][:, 0:128], self.ones32[:], zf, start=True, stop=True),
                    reads=(s_z, self.s_const), writes=(self.pbs[1],))
            tot = self.sb(p3, "tot", [128, NT, 8], F32)
            toff = self.sb(p3, "toff", [128, NT, 8], F32)
            s_tot, s_toff = Slot(), Slot()
            self.op("dve", lambda E: E.tensor_copy(tot[:].rearrange("p t h -> p (t h)"), self.pb[1][:, 0:128]),
                    reads=(self.pbs[1],), writes=(s_tot,))
            self.op("dve", lambda E: E.memset(toff[:, 0, :], 0.0), writes=(s_toff,))
            for t in range(1, NT):
                self.op("dve", lambda E: E.tensor_tensor(out=toff[:, t, :], in0=toff[:, t - 1, :], in1=tot[:, t - 1, :],
                                                         op=ALU.add), reads=(s_tot,), writes=(s_toff,))
            self.op("dve", lambda E: E.tensor_tensor(out=Fp[:].rearrange("p t h -> p (t h)"), in0=self.pb[0][:, 0:128],
                                                     in1=toff[:].rearrange("p t h -> p (t h)"), op=ALU.add),
                    reads=(self.pbs[0], s_toff), writes=(s_Fp,))
            if self.dbg and l == 0:
                self.dma("sp", self.dbg_F, Fp[:], reads=(s_Fp,))
            self.barrier()
        wqk = [self.sb(ph, f"wqk{i}", [128, 2, KC, 128], BF16) for i in range(2)]
        s_wqk = [Slot(), Slot()]
        qT = self.sb(ph, "qT", [128, S], BF16)
        kT = self.sb(ph, "kT", [128, S], BF16)
        s_q, s_k = Slot(), Slot()
        Ftb = self.sb(ph, "Ftb", [128, S], F32)
        s_Ftb = Slot()
        diag = [self.sb(ph, f"diag{i}", [128, 128], F32) for i in range(2)]
        s_diag = [Slot(), Slot()]
        tmp = [self.sb(ph, f"atmp{i}", [128, 512], F32) for i in range(2)]
        s_tmp = [Slot() for _ in range(2)]
        P = [self.sb(ph, f"P{i}", [128, 512], BF16) for i in range(4)]
        s_P = [Slot() for _ in range(4)]
        self._att_it = 0
        rden = self.sb(ph, "rden", [128, 512], F32)
        s_rden = Slot()
        ip = 0
        it = 0
        for h in range(8):
            r = h % 2
            self.dma("pool", wqk[r][:, 0],
                     self.w_in[l, :, 1024 + h * 128:1024 + (h + 1) * 128].rearrange("(k p) n -> p k n", p=128),
                     writes=(s_wqk[r],))
            self.dma("pool", wqk[r][:, 1],
                     self.w_in[l, :, 2048 + h * 128:2048 + (h + 1) * 128].rearrange("(k p) n -> p k n", p=128),
                     writes=(s_wqk[r],))
            for which, dst, s_dst in ((0, qT, s_q), (1, kT, s_k)):
                for tt in range(4):
                    bk = tt % 2
                    for k in range(KC):
                        self.op("pe", lambda E: E.matmul(self.pb[bk][:], wqk[r][:, which, k, :],
                                                         hT[:, k, tt * 512:(tt + 1) * 512],
                                                         start=(k == 0), stop=(k == KC - 1)),
                                reads=(s_wqk[r], s_hT), writes=(self.pbs[bk],), signal=(k == KC - 1))
                    if which == 0:
                        self.op("act", lambda E: E.activation(out=dst[:, tt * 512:(tt + 1) * 512], in_=self.pb[bk][:],
                                                              func=AF.Copy), reads=(self.pbs[bk],), writes=(s_dst,))
                    else:
                        self.op("dve", lambda E: E.tensor_copy(dst[:, tt * 512:(tt + 1) * 512], self.pb[bk][:]),
                                reads=(self.pbs[bk],), writes=(s_dst,))
            for t in range(NT):
                d = t % 2
                bk = (t // 4) % 2
                self.op("dve", lambda E: E.tensor_scalar(out=diag[d][:], in0=self.ident32[:], scalar1=Fp[:, t, h:h + 1],
                                                         scalar2=-1.0, op0=ALU.mult, op1=ALU.mult),
                        reads=(s_Fp, self.s_const), writes=(s_diag[d],))
                self.op("pe", lambda E: E.matmul(self.pb[bk][:, (t % 4) * 128:(t % 4 + 1) * 128], self.ones32[:], diag[d][:],
                                                 start=True, stop=True),
                        reads=(s_diag[d], self.s_const), writes=(self.pbs[bk],), signal=True)
                if t % 4 == 3:
                    self.op("act", lambda E: E.activation(out=Ftb[:, (t - 3) * 128:(t + 1) * 128], in_=self.pb[bk][:],
                                                          func=AF.Copy), reads=(self.pbs[bk],), writes=(s_Ftb,))
            tiles = []
            for j in range(4):
                nk = 4 * j + 4
                for kc in range(nk):
                    tiles.append((j, kc, nk))
            SB = (2, 3, 4, 7)
            LA = 3
            meta = {}

            def emit_S(i):
                j, kc, nk = tiles[i]
                q0 = 0 if kc < 4 * j else 128 * (kc - 4 * j)
                ncol = 512 - q0
                qs = j * 512 + q0
                nonlocal_it = self._att_it
                self._att_it += 1
                bS = SB[nonlocal_it % 4]
                tb = nonlocal_it % 2
                pi = nonlocal_it % 4
                meta[i] = (q0, ncol, pi)
                self.op("pe", lambda E: E.matmul(self.pb[bS][:, 0:ncol], kT[:, kc * 128:(kc + 1) * 128],
                                                 qT[:, qs:qs + ncol], start=True, stop=True),
                        reads=(s_k, s_q), writes=(self.pbs[bS],))
                self.op("dve", lambda E: E.scalar_tensor_tensor(out=tmp[tb][:, 0:ncol], in0=self.pb[bS][:, 0:ncol],
                                                                scalar=scale, in1=Ftb[:, qs:qs + ncol],
                                                                op0=ALU.mult, op1=ALU.add),
                        reads=(self.pbs[bS], s_Ftb), writes=(s_tmp[tb],))
                self.op("act", lambda E: E.activation(out=P[pi][:, 0:ncol], in_=tmp[tb][:, 0:ncol], func=AF.Exp,
                                                      bias=Fp[:, kc, h:h + 1], scale=1.0),
                        reads=(s_tmp[tb], s_Fp), writes=(s_P[pi],))
                if kc >= 4 * j:
                    self.op("dve", lambda E: E.tensor_tensor(out=P[pi][:, 0:128], in0=P[pi][:, 0:128],
                                                             in1=self.tribf[:], op=ALU.mult),
                            reads=(self.s_const,), writes=(s_P[pi],))

            def emit_PV(i):
                j, kc, nk = tiles[i]
                q0, ncol, pi = meta.pop(i)
                bO, bD = 5, 6
                self.op("pe", lambda E: E.matmul(self.pb[bO][:, q0:512], V[:, kc, h * 128:(h + 1) * 128],
                                                 P[pi][:, 0:ncol], start=(kc == 0), stop=(kc == nk - 1)),
                        reads=(s_V, s_P[pi]), writes=(self.pbs[bO],), signal=False)
                self.op("pe", lambda E: E.matmul(self.pb[bD][:, q0:512], self.onesbf[:], P[pi][:, 0:ncol],
                                                 start=(kc == 0), stop=(kc == nk - 1)),
                        reads=(self.s_const, s_P[pi]), writes=(self.pbs[bD],), signal=True)
                if kc == nk - 1:
                    self.op("dve", lambda E: E.reciprocal(out=rden[:], in_=self.pb[bD][:]), reads=(self.pbs[bD],),
                            writes=(s_rden,))
                    self.op("dve", lambda E: E.tensor_tensor(out=attn_out[:, h, j * 512:(j + 1) * 512], in0=self.pb[bO][:],
                                                             in1=rden[:], op=ALU.mult),
                            reads=(self.pbs[bO], s_rden), writes=(s_ao,))

            n = len(tiles)
            for i in range(n + LA):
                if i < n:
                    emit_S(i)
                if i - LA >= 0:
                    emit_PV(i - LA)

    def phase_merge(self, ph, l, hT, s_hT, pool_out, s_po, attn_out, s_ao):
        wsl = [self.sb(ph, f"wmg{i}", [128, 48, 128], BF16) for i in range(3)]
        s_w = [Slot() for _ in range(3)]
        bg = self.sb(ph, "bg", [128, 32], F32)
        s_bg = Slot()
        self.dma("sp", bg[:], self.b_gate[l:l + 1, :].rearrange("o (c p) -> p (o c)", p=128), writes=(s_bg,),
                 allow_slow_non_contiguous=True)
        sg = [self.sb(ph, f"sg{i}", [128, 512], F32) for i in range(4)]
        s_sg = [Slot() for _ in range(4)]
        mt = [self.sb(ph, f"mt{i}", [128, S], BF16) for i in range(2)]
        s_mt = [Slot(), Slot()]
        it = 0
        for c in range(KC):
            r = c % 3
            cs = slice(c * 128, (c + 1) * 128)
            self.dma("pool", wsl[r][:, 0:8, :], self.w_branch[l, 0, :, cs].rearrange("(k p) n -> p k n", p=128),
                     writes=(s_w[r],))
            self.dma("pool", wsl[r][:, 8:16, :], self.w_branch[l, 1, :, cs].rearrange("(k p) n -> p k n", p=128),
                     writes=(s_w[r],))
            self.dma("pool", wsl[r][:, 16:32, :], self.w_gate[l, :, cs].rearrange("(k p) n -> p k n", p=128),
                     writes=(s_w[r],))
            self.dma("pool", wsl[r][:, 32:48, :],
                     self.w_gate[l, :, D + c * 128:D + (c + 1) * 128].rearrange("(k p) n -> p k n", p=128),
                     writes=(s_w[r],))
            m = c % 2
            for tt in range(4):
                ts = slice(tt * 512, (tt + 1) * 512)
                b0 = 4 * (it % 2)
                so = 2 * (it % 2)
                it += 1
                for k in range(8):
                    self.op("pe", lambda E: E.matmul(self.pb[b0][:], wsl[r][:, k, :], pool_out[:, k, ts],
                                                     start=(k == 0), stop=(k == 7)),
                            reads=(s_w[r], s_po), writes=(self.pbs[b0],), signal=(k == 7))
                for k in range(8):
                    self.op("pe", lambda E: E.matmul(self.pb[b0 + 1][:], wsl[r][:, 8 + k, :], attn_out[:, k, ts],
                                                     start=(k == 0), stop=(k == 7)),
                            reads=(s_w[r], s_ao), writes=(self.pbs[b0 + 1],), signal=(k == 7))
                for gi in range(2):
                    for k in range(KC):
                        self.op("pe", lambda E: E.matmul(self.pb[b0 + 2 + gi][:], wsl[r][:, 16 + 16 * gi + k, :], hT[:, k, ts],
                                                         start=(k == 0), stop=(k == KC - 1)),
                                reads=(s_w[r], s_hT), writes=(self.pbs[b0 + 2 + gi],), signal=(k == KC - 1))
                for gi in range(2):
                    self.op("act", lambda E: E.activation(out=sg[so + gi][:], in_=self.pb[b0 + 2 + gi][:], func=AF.Sigmoid,
                                                          bias=bg[:, 16 * gi + c:16 * gi + c + 1], scale=1.0),
                            reads=(self.pbs[b0 + 2 + gi], s_bg), writes=(s_sg[so + gi],))
                for gi in range(2):
                    self.op("dve", lambda E: E.tensor_tensor(out=sg[so + gi][:], in0=self.pb[b0 + gi][:], in1=sg[so + gi][:],
                                                             op=ALU.mult),
                            reads=(self.pbs[b0 + gi],), writes=(s_sg[so + gi],))
                self.op("dve", lambda E: E.tensor_tensor(out=mt[m][:, ts], in0=sg[so][:], in1=sg[so + 1][:], op=ALU.add),
                        reads=(s_sg[so], s_sg[so + 1]), writes=(s_mt[m],))
            self.dma("sp", self.mT_d[:, :, c, :].rearrange("t p tok -> p t tok"), mt[m][:].rearrange("p (t tok) -> p t tok", tok=128), reads=(s_mt[m],))

    def phase_wout(self, ph, l, x_src):
        wo = self.sb(ph, "wo", [128, KC, D], BF16)
        s_wo = Slot()
        for q in range(8):
            self.dma("pool", wo[:, 2 * q:2 * q + 2, :],
                     self.w_out[l, q * 256:(q + 1) * 256, :].rearrange("(k p) n -> p k n", p=128), writes=(s_wo,))
        G = self.sb(ph, "G", [128, D], F32)
        s_G = Slot()
        self.dma("sp", G[:], self.mod_d[l:l + 1, 2 * D:3 * D].partition_broadcast(128), writes=(s_G,))
        mt = [self.sb(ph, f"mtl{i}", [128, KC, 128], BF16) for i in range(3)]
        s_mt = [Slot() for _ in range(3)]
        xt = [self.sb(ph, f"xw{i}", [128, D], F32) for i in range(2)]
        s_xt = [Slot(), Slot()]
        t1 = [self.sb(ph, f"t1w{i}", [128, 512], F32) for i in range(2)]
        s_t1 = [Slot(), Slot()]
        xn = [self.sb(ph, f"xn{i}", [128, D], F32) for i in range(2)]
        s_xn = [Slot(), Slot()]

        def load(t):
            self.dma("sp", mt[t % 3][:], self.mT_d[t], writes=(s_mt[t % 3],))
            self.dma("sp", xt[t % 2][:], x_src[t * 128:(t + 1) * 128, :], writes=(s_xt[t % 2],))

        load(0)
        i1 = 0
        for t in range(NT):
            if t + 1 < NT:
                load(t + 1)
            b = t % 2
            for k in range(KC):
                for n in range(4):
                    bk = 4 * (t % 2) + n
                    self.op("pe", lambda E: E.matmul(self.pb[bk][:], mt[t % 3][:, k, :], wo[:, k, n * 512:(n + 1) * 512],
                                                     start=(k == 0), stop=(k == KC - 1)),
                            reads=(s_mt[t % 3], s_wo), writes=(self.pbs[bk],), signal=(k == KC - 1))
            for n in range(4):
                bk = 4 * (t % 2) + n
                ns = slice(n * 512, (n + 1) * 512)
                q = i1 % 2
                i1 += 1
                self.op("dve", lambda E: E.tensor_tensor(out=t1[q][:], in0=self.pb[bk][:], in1=G[:, ns], op=ALU.mult),
                        reads=(self.pbs[bk], s_G), writes=(s_t1[q],))
                self.op("dve", lambda E: E.tensor_tensor(out=xn[b][:, ns], in0=t1[q][:], in1=xt[b][:, ns], op=ALU.add),
                        reads=(s_t1[q], s_xt[b]), writes=(s_xn[b],))
            self.dma("sp", self.xres[t * 128:(t + 1) * 128, :], xn[b][:], reads=(s_xn[b],))

    def layer_moe(self, l):
        if SPARSE:
            return self.layer_moe_sparse(l)
        with contextlib.ExitStack() as ph:
            hT = self.sb(ph, "h2T", [128, KC, S], BF16)
            s_hT = Slot()
            wr32 = self.sb(ph, "wr32", [128, KC, NE], F32)
            wr = self.sb(ph, "wr", [128, 2, KC, NE], BF16)
            wtmp = self.sb(ph, "wtmp", [128, KC, NE], F32)
            s_wr = Slot()
            for k in range(KC):
                self.dma("sp", wr32[:, k, :], self.w_router[k * 128:(k + 1) * 128, :], writes=(s_wr,))
            self.op("dve", lambda E: E.tensor_copy(wr[:, 0], wr32[:]), writes=(s_wr,))
            self.op("dve", lambda E: E.tensor_tensor(out=wtmp[:], in0=wr32[:], in1=wr[:, 0], op=ALU.subtract), writes=(s_wr,))
            self.op("dve", lambda E: E.tensor_copy(wr[:, 1], wtmp[:]), writes=(s_wr,))
            lg = self.sb(ph, "lg", [128, NT, NE], F32)
            s_lg = Slot()
            with contextlib.ExitStack() as ph2:
                import os
                self.phase_norm(ph2, l, self.xres, self.norm_moe[l:l + 1, :], 3 * D, 4 * D, hT, s_hT,
                                router=(None if os.environ.get("NOROUTER") == "1" else (wr, s_wr, lg, s_lg)))
            self.barrier()
            for q in range(4):
                self.dma("sp", self.h2T_d[:, q * 4:(q + 1) * 4, :], hT[:, q * 4:(q + 1) * 4, :], reads=(s_hT,))
            import os
            if os.environ.get('SKIPGATE') != '1':
                self.phase_gating(ph, lg, s_lg)
        self.barrier()
        if self.stop_after == f"gate{l}":
            return
        for half in range(2):
            with contextlib.ExitStack() as ph:
                self.phase_experts(ph, l, half)
            self.barrier()

    def phase_gating(self, ph, lg, s_lg, sparse=None):
        def T(name, shape):
            return self.sb(ph, name, shape, F32)
        s = s_lg
        mx = T("mx", [128, NT])
        sm = T("sm", [128, NT])
        pr = T("pr", [128, NT, NE])
        sel = T("sel", [128, NT, NE])
        brb = T("brb", [128, NE])
        psum6 = T("psum6", [128, NT, 4, 6])
        ind = T("ind", [128, NT, 4, 6])
        gmx = T("gmx", [128, NT])
        mask = T("mask", [128, NT, NE])
        gw = T("gw", [128, NT, NE])
        gs = T("gs", [128, NT])
        gwT = T("gwT", [NE, S])
        s_b = Slot()
        s_gwT = Slot()
        self.dma("sp", brb[:], self.b_router.partition_broadcast(128), writes=(s_b,))

        def dve(fn, extra_reads=()):
            return self.op("dve", fn, reads=extra_reads, writes=(s,))

        def bc(ap2):
            return ap2.unsqueeze(2).to_broadcast([128, NT, NE])

        dve(lambda E: E.tensor_reduce(out=mx[:], in_=lg[:], axis=AX.X, op=ALU.max))
        dve(lambda E: E.tensor_tensor(out=pr[:], in0=lg[:], in1=bc(mx[:]), op=ALU.subtract))
        self.op("act", lambda E: E.activation(out=pr[:], in_=pr[:], func=AF.Exp), writes=(s,))
        dve(lambda E: E.tensor_reduce(out=sm[:], in_=pr[:], axis=AX.X, op=ALU.add))
        dve(lambda E: E.reciprocal(out=sm[:], in_=sm[:]))
        dve(lambda E: E.tensor_tensor(out=pr[:], in0=pr[:], in1=bc(sm[:]), op=ALU.mult))
        dve(lambda E: E.tensor_tensor(out=sel[:], in0=pr[:], in1=brb[:].unsqueeze(1).to_broadcast([128, NT, NE]),
                                      op=ALU.add), extra_reads=(s_b,))
        sel4 = sel[:].rearrange("p t (g i) -> p t g i", i=4)
        pairs = [(0, 1), (0, 2), (0, 3), (1, 2), (1, 3), (2, 3)]
        for pi, (a, b) in enumerate(pairs):
            dve(lambda E: E.tensor_tensor(out=psum6[:, :, :, pi], in0=sel4[:, :, :, a], in1=sel4[:, :, :, b], op=ALU.add))
        dve(lambda E: E.tensor_reduce(out=gmx[:], in_=psum6[:].rearrange("p t g s -> p t (g s)"), axis=AX.X, op=ALU.max))
        dve(lambda E: E.tensor_tensor(out=ind[:].rearrange("p t g s -> p t (g s)"),
                                      in0=psum6[:].rearrange("p t g s -> p t (g s)"),
                                      in1=gmx[:].unsqueeze(2).to_broadcast([128, NT, 24]), op=ALU.is_equal))
        mask4 = mask[:].rearrange("p t (g i) -> p t g i", i=4)
        member = {0: (0, 1, 2), 1: (0, 3, 4), 2: (1, 3, 5), 3: (2, 4, 5)}
        for i in range(4):
            p0, p1, p2 = member[i]
            dve(lambda E: E.tensor_tensor(out=mask4[:, :, :, i], in0=ind[:, :, :, p0], in1=ind[:, :, :, p1], op=ALU.add))
            dve(lambda E: E.tensor_tensor(out=mask4[:, :, :, i], in0=mask4[:, :, :, i], in1=ind[:, :, :, p2], op=ALU.add))
        dve(lambda E: E.tensor_tensor(out=gw[:], in0=pr[:], in1=mask[:], op=ALU.mult))
        dve(lambda E: E.tensor_reduce(out=gs[:], in_=gw[:], axis=AX.X, op=ALU.add))
        dve(lambda E: E.reciprocal(out=gs[:], in_=gs[:]))
        dve(lambda E: E.tensor_tensor(out=gw[:], in0=gw[:], in1=bc(gs[:]), op=ALU.mult))
        if sparse is not None:
            return self.phase_positions(ph, sparse, s, mask, gw, dve, T)
        for t in range(NT):
            bk = (t // 4) % 2
            self.op("pe", lambda E: E.transpose(self.pb[bk][0:NE, (t % 4) * 128:(t % 4 + 1) * 128], gw[:, t, :],
                                                self.ident32[:]),
                    reads=(s, self.s_const), writes=(self.pbs[bk],), signal=True)
            if t % 4 == 3:
                self.op("act", lambda E: E.activation(out=gwT[:, (t - 3) * 128:(t + 1) * 128], in_=self.pb[bk][0:NE, :],
                                                      func=AF.Copy), reads=(self.pbs[bk],), writes=(s_gwT,))
        self.dma("sp", self.gwT_d, gwT[:], reads=(s_gwT,))

    def phase_experts(self, ph, l, half):
        acc = self.sb(ph, "acc", [128, 8, D], F32)
        s_acc = Slot()
        self._experts_inner(l, half, acc, s_acc)
        self.barrier()
        G2 = self.sb(ph, "G2", [128, D], F32)
        s_G2 = Slot()
        self.dma("sp", G2[:], self.mod_d[l:l + 1, 5 * D:6 * D].partition_broadcast(128), writes=(s_G2,))
        xt = [self.sb(ph, f"xm{i}", [128, D], F32) for i in range(2)]
        s_xt = [Slot(), Slot()]
        for sub in range(8):
            t = half * 8 + sub
            b = sub % 2
            self.dma("sp", xt[b][:], self.xres[t * 128:(t + 1) * 128, :], writes=(s_xt[b],))
            self.op("dve", lambda E: E.tensor_tensor(out=acc[:, sub, :], in0=acc[:, sub, :], in1=G2[:], op=ALU.mult),
                    reads=(s_G2,), writes=(s_acc,))
            self.op("dve", lambda E: E.tensor_tensor(out=xt[b][:], in0=acc[:, sub, :], in1=xt[b][:], op=ALU.add),
                    reads=(s_acc,), writes=(s_xt[b],))
            self.dma("sp", self.xres[t * 128:(t + 1) * 128, :], xt[b][:], reads=(s_xt[b],))

    def _experts_inner(self, l, half, acc, s_acc):
      with contextlib.ExitStack() as ph:
        HS = S // 2
        t0 = half * HS
        h2 = self.sb(ph, "h2", [128, KC, HS], BF16)
        s_h2 = Slot()
        for q in range(4):
            self.dma("sp", h2[:, q * 4:(q + 1) * 4, :], self.h2T_d[:, q * 4:(q + 1) * 4, t0:t0 + HS], writes=(s_h2,))
        actp = self.sb(ph, "actp", [128, 8, HS], BF16)
        s_actp = Slot()
        wd = self.sb(ph, "wd", [128, 8, D], BF16)
        s_wd = Slot()
        wgu = [self.sb(ph, f"wgu{i}", [128, 2, KC, 128], BF16) for i in range(3)]
        s_wgu = [Slot() for _ in range(3)]
        gwb = [self.sb(ph, f"gwb{i}", [128, HS], F32) for i in range(2)]
        s_gwb = [Slot(), Slot()]
        sa = [self.sb(ph, f"sa{i}", [128, 512], F32) for i in range(2)]
        s_sa = [Slot(), Slot()]
        iw = 0
        ia = 0
        for e in range(self.nexp):
            ge = e % 2
            self.dma("sp", gwb[ge][:], self.gwT_d[e:e + 1, t0:t0 + HS].partition_broadcast(128), writes=(s_gwb[ge],))
            for j in range(8):
                r = iw % 3
                iw += 1
                cs = slice(j * 128, (j + 1) * 128)
                self.dma("pool", wgu[r][:, 0], self.w_eg[l, e, :, cs].rearrange("(k p) n -> p k n", p=128),
                         writes=(s_wgu[r],))
                self.dma("pool", wgu[r][:, 1], self.w_eu[l, e, :, cs].rearrange("(k p) n -> p k n", p=128),
                         writes=(s_wgu[r],))
                for tt in range(2):
                    ts = slice(tt * 512, (tt + 1) * 512)
                    b0 = 2 * (ia % 2)
                    q = ia % 2
                    ia += 1
                    for which in range(2):
                        for k in range(KC):
                            self.op("pe", lambda E: E.matmul(self.pb[b0 + which][:], wgu[r][:, which, k, :], h2[:, k, ts],
                                                             start=(k == 0), stop=(k == KC - 1)),
                                    reads=(s_wgu[r], s_h2), writes=(self.pbs[b0 + which],), signal=(k == KC - 1))
                    self.op("act", lambda E: E.activation(out=sa[q][:], in_=self.pb[b0][:], func=AF.Silu),
                            reads=(self.pbs[b0],), writes=(s_sa[q],))
                    self.op("dve", lambda E: E.tensor_tensor(out=sa[q][:], in0=self.pb[b0 + 1][:], in1=sa[q][:], op=ALU.mult),
                            reads=(self.pbs[b0 + 1],), writes=(s_sa[q],))
                    self.op("dve", lambda E: E.tensor_tensor(out=actp[:, j, ts], in0=sa[q][:], in1=gwb[ge][:, ts], op=ALU.mult),
                            reads=(s_gwb[ge],), writes=(s_actp, s_sa[q]))
            for q in range(4):
                self.dma("pool", wd[:, 2 * q:2 * q + 2, :],
                         self.w_ed[l, e, q * 256:(q + 1) * 256, :].rearrange("(k p) n -> p k n", p=128), writes=(s_wd,))
            for sub in range(8):
                for n in range(4):
                    bk = 4 + n
                    for k in range(8):
                        self.op("pe", lambda E: E.matmul(self.pb[bk][:], actp[:, k, sub * 128:(sub + 1) * 128],
                                                         wd[:, k, n * 512:(n + 1) * 512], start=(k == 0), stop=(k == 7)),
                                reads=(s_actp, s_wd), writes=(self.pbs[bk],), signal=(k == 7))
                    ns = slice(n * 512, (n + 1) * 512)
                    if e == 0:
                        self.op("dve", lambda E: E.tensor_copy(acc[:, sub, ns], self.pb[bk][:]), reads=(self.pbs[bk],),
                                writes=(s_acc,))
                    else:
                        self.op("dve", lambda E: E.tensor_tensor(out=acc[:, sub, ns], in0=self.pb[bk][:],
                                                                 in1=acc[:, sub, ns], op=ALU.add),
                                reads=(self.pbs[bk],), writes=(s_acc,))

    def idma(self, out, in_, idx_ap, gather, reads=(), writes=(), extra=(), **kw):
        self._wait("pool", self._deps(reads, writes, extra))
        i = self._next_dsem("pool")
        self.dcnt[i] += 1
        off = bass.IndirectOffsetOnAxis(ap=idx_ap, axis=0)
        if gather:
            ins = self.nc.gpsimd.indirect_dma_start(out=out, out_offset=None, in_=in_, in_offset=off, **kw)
        else:
            ins = self.nc.gpsimd.indirect_dma_start(out=out, out_offset=off, in_=in_, in_offset=None, **kw)
        ins.then_inc(self.semh[f"d{i}"], 16)
        tok = (f"d{i}", 16 * self.dcnt[i])
        for sl in reads:
            if sl.r.get(tok[0], 0) < tok[1]:
                sl.r[tok[0]] = tok[1]
        for sl in writes:
            sl.w = tok
            sl.r = {}
        return tok

    def phase_positions(self, ph, sp, s, mask, gw, dve, T):
        maskb = self.sb(ph, "maskb", [128, NT * NE], BF16)
        rank = T("rank", [128, NT, NE])
        cnt = T("cnt", [128, NT, NE])
        toff = T("toff", [128, NT, NE])
        total = T("total", [128, NE])
        thr = T("thr", [128, NE, 16])
        cmp = T("cmp", [128, NE, 16])
        nblk = T("nblk", [128, NE])
        pend = T("pend", [128, NE])
        pstart = T("pstart", [128, NE])
        dest = T("dest", [128, NT, NE])
        destm = T("destm", [128, NT, NE])
        tmpm = T("tmpm", [128, NT, NE])
        dlo = T("dlo", [128, NT])
        dhi = T("dhi", [128, NT])
        bvals = T("bvals", [128, NBLK, NE])
        cmpb = T("cmpb", [128, NBLK, NE])
        be = T("be", [128, NBLK])
        prev = T("prev", [128, NBLK])
        chg = T("chg", [128, NBLK])
        t2 = T("t2", [128, NBLK])
        idxf = T("idxf", [128, NBLK])
        pcol = T("pcol", [128, 1])
        maskf = mask[:].rearrange("p t e -> p (t e)")
        dve(lambda E: E.tensor_copy(maskb[:], maskf))
        self.op("pe", lambda E: E.matmul(self.pb[0][:, 0:NT * NE], self.ustrbf[:], maskb[:], start=True, stop=True),
                reads=(s, self.s_const), writes=(self.pbs[0],))
        self.op("pe", lambda E: E.matmul(self.pb[1][:, 0:NT * NE], self.onesbf[:], maskb[:], start=True, stop=True),
                reads=(s, self.s_const), writes=(self.pbs[1],))
        self.op("pe", lambda E: E.matmul(self.pb[2][:, 0:1], self.ustrbf[:], self.onesbf[:, 0:1], start=True, stop=True),
                reads=(self.s_const,), writes=(self.pbs[2],))
        dve(lambda E: E.tensor_copy(rank[:].rearrange("p t e -> p (t e)"), self.pb[0][:, 0:NT * NE]), (self.pbs[0],))
        dve(lambda E: E.tensor_copy(cnt[:].rearrange("p t e -> p (t e)"), self.pb[1][:, 0:NT * NE]), (self.pbs[1],))
        dve(lambda E: E.tensor_copy(pcol[:], self.pb[2][:, 0:1]), (self.pbs[2],))
        dve(lambda E: E.memset(toff[:, 0, :], 0.0))
        for t in range(1, NT):
            dve(lambda E: E.tensor_tensor(out=toff[:, t, :], in0=toff[:, t - 1, :], in1=cnt[:, t - 1, :], op=ALU.add))
        dve(lambda E: E.tensor_tensor(out=total[:], in0=toff[:, NT - 1, :], in1=cnt[:, NT - 1, :], op=ALU.add))
        for m in range(16):
            dve(lambda E: E.memset(thr[:, :, m], 128.0 * m))
        dve(lambda E: E.tensor_tensor(out=cmp[:], in0=total[:].unsqueeze(2).to_broadcast([128, NE, 16]), in1=thr[:],
                                      op=ALU.is_gt))
        dve(lambda E: E.tensor_reduce(out=nblk[:], in_=cmp[:], axis=AX.X, op=ALU.add))
        dve(lambda E: E.tensor_copy(pend[:, 0:1], nblk[:, 0:1]))
        for e in range(1, NE):
            dve(lambda E: E.tensor_tensor(out=pend[:, e:e + 1], in0=pend[:, e - 1:e], in1=nblk[:, e:e + 1], op=ALU.add))
        dve(lambda E: E.tensor_tensor(out=pstart[:], in0=pend[:], in1=nblk[:], op=ALU.subtract))
        dve(lambda E: E.tensor_scalar(out=pstart[:], in0=pstart[:], scalar1=128.0, scalar2=None, op0=ALU.mult))
        dve(lambda E: E.tensor_tensor(out=dest[:], in0=rank[:], in1=toff[:], op=ALU.add))
        dve(lambda E: E.tensor_tensor(out=dest[:], in0=dest[:], in1=pstart[:].unsqueeze(1).to_broadcast([128, NT, NE]),
                                      op=ALU.add))
        dve(lambda E: E.tensor_scalar(out=tmpm[:], in0=mask[:], scalar1=-BIGIDX, scalar2=BIGIDX, op0=ALU.mult, op1=ALU.add))
        dve(lambda E: E.tensor_tensor(out=destm[:], in0=dest[:], in1=tmpm[:], op=ALU.add))
        dve(lambda E: E.tensor_reduce(out=dlo[:], in_=destm[:], axis=AX.X, op=ALU.min))
        dve(lambda E: E.tensor_tensor(out=tmpm[:], in0=dest[:], in1=mask[:], op=ALU.mult))
        dve(lambda E: E.tensor_reduce(out=dhi[:], in_=tmpm[:], axis=AX.X, op=ALU.max))
        dve(lambda E: E.tensor_tensor(out=tmpm[:], in0=destm[:], in1=dlo[:].unsqueeze(2).to_broadcast([128, NT, NE]),
                                      op=ALU.is_equal))
        dve(lambda E: E.tensor_tensor(out=tmpm[:], in0=tmpm[:], in1=gw[:], op=ALU.mult))
        dve(lambda E: E.tensor_reduce(out=sp["wlo"][:], in_=tmpm[:], axis=AX.X, op=ALU.add))
        dve(lambda E: E.tensor_scalar(out=sp["whi"][:], in0=sp["wlo"][:], scalar1=-1.0, scalar2=1.0, op0=ALU.mult,
                                      op1=ALU.add))
        dve(lambda E: E.tensor_copy(sp["dlo_i"][:], dlo[:]))
        dve(lambda E: E.tensor_copy(sp["dhi_i"][:], dhi[:]))
        for b in range(NBLK):
            dve(lambda E: E.memset(bvals[:, b, :], float(b)))
        dve(lambda E: E.tensor_tensor(out=cmpb[:], in0=pend[:].unsqueeze(1).to_broadcast([128, NBLK, NE]), in1=bvals[:],
                                      op=ALU.is_le))
        dve(lambda E: E.tensor_reduce(out=be[:], in_=cmpb[:], axis=AX.X, op=ALU.add))
        dve(lambda E: E.tensor_scalar(out=be[:], in0=be[:], scalar1=float(NE - 1), scalar2=None, op0=ALU.min))
        dve(lambda E: E.memset(prev[:, 0:1], -1.0))
        dve(lambda E: E.tensor_copy(prev[:, 1:NBLK], be[:, 0:NBLK - 1]))
        dve(lambda E: E.tensor_tensor(out=chg[:], in0=be[:], in1=prev[:], op=ALU.not_equal))
        if not SKIP:
            dve(lambda E: E.memset(chg[:], 1.0))
        dve(lambda E: E.tensor_scalar(out=t2[:], in0=chg[:], scalar1=-BIGIDX, scalar2=BIGIDX, op0=ALU.mult, op1=ALU.add))
        for name, rows in (("idxW_i", 2048.0), ("idxD_i", 1024.0)):
            dve(lambda E: E.tensor_scalar(out=idxf[:], in0=be[:], scalar1=rows, scalar2=pcol[:, 0:1], op0=ALU.mult,
                                          op1=ALU.add))
            dve(lambda E: E.tensor_tensor(out=idxf[:], in0=idxf[:], in1=chg[:], op=ALU.mult))
            dve(lambda E: E.tensor_tensor(out=idxf[:], in0=idxf[:], in1=t2[:], op=ALU.add))
            dve(lambda E: E.tensor_copy(sp[name][:], idxf[:]))
        if self.dbg:
            dbgt = T("dbgt", [128, 4 * NT + 2 * NBLK])
            dve(lambda E: E.tensor_copy(dbgt[:, 0:NT], dlo[:]))
            dve(lambda E: E.tensor_copy(dbgt[:, NT:2 * NT], dhi[:]))
            dve(lambda E: E.tensor_copy(dbgt[:, 2 * NT:3 * NT], sp["wlo"][:]))
            dve(lambda E: E.tensor_copy(dbgt[:, 3 * NT:4 * NT], sp["whi"][:]))
            dve(lambda E: E.tensor_copy(dbgt[:, 4 * NT:4 * NT + NBLK], be[:]))
            dve(lambda E: E.tensor_copy(dbgt[:, 4 * NT + NBLK:4 * NT + 2 * NBLK], idxf[:]))
            self.dma("sp", self.dbg_idx, dbgt[:], reads=(s,))

    def layer_moe_sparse(self, l):
        with contextlib.ExitStack() as pl:
            sp = dict(
                wlo=self.sb(pl, "wlo", [128, NT], F32), whi=self.sb(pl, "whi", [128, NT], F32),
                dlo_i=self.sb(pl, "dlo_i", [128, NT], I32), dhi_i=self.sb(pl, "dhi_i", [128, NT], I32),
                idxW_i=self.sb(pl, "idxW_i", [128, NBLK], I32), idxD_i=self.sb(pl, "idxD_i", [128, NBLK], I32))
            s_sp = Slot()
            with contextlib.ExitStack() as ph:
                h2tm = self.sb(ph, "h2tm", [128, NT, D], BF16)
                s_h2tm = Slot()
                hT = self.sb(ph, "hhi", [128, KC, 128], BF16)
                s_hT = Slot()
                wr32 = self.sb(ph, "wr32", [128, KC, NE], F32)
                wr = self.sb(ph, "wr", [128, 2, KC, NE], BF16)
                wtmp = self.sb(ph, "wtmp", [128, KC, NE], F32)
                s_wr = Slot()
                for k in range(KC):
                    self.dma("sp", wr32[:, k, :], self.w_router[k * 128:(k + 1) * 128, :], writes=(s_wr,))
                self.op("dve", lambda E: E.tensor_copy(wr[:, 0], wr32[:]), writes=(s_wr,))
                self.op("dve", lambda E: E.tensor_tensor(out=wtmp[:], in0=wr32[:], in1=wr[:, 0], op=ALU.subtract),
                        writes=(s_wr,))
                self.op("dve", lambda E: E.tensor_copy(wr[:, 1], wtmp[:]), writes=(s_wr,))
                lg = self.sb(ph, "lg", [128, NT, NE], F32)
                s_lg = Slot()
                with contextlib.ExitStack() as ph2:
                    self.phase_norm(ph2, l, self.xres, self.norm_moe[l:l + 1, :], 3 * D, 4 * D, hT, s_hT,
                                    router=(wr, s_wr, lg, s_lg), tm_out=(h2tm, s_h2tm))
                self.barrier()
                with contextlib.ExitStack() as ph2:
                    self.phase_gating(ph2, lg, s_lg, sparse=sp)
                self.barrier()
                for t in range(NT):
                    self.idma(self.buf_d, h2tm[:, t, :], sp["dlo_i"][:, t:t + 1], gather=False, reads=(s_h2tm,))
                    self.idma(self.buf_d, h2tm[:, t, :], sp["dhi_i"][:, t:t + 1], gather=False, reads=(s_h2tm,))
            self.barrier()
            if self.stop_after == f"gate{l}":
                return
            with contextlib.ExitStack() as ph:
                self.phase_blocks(ph, l, sp)
            self.barrier()
            with contextlib.ExitStack() as ph:
                self.phase_combine(ph, l, sp)
            self.barrier()

    def phase_blocks(self, ph, l, sp):
        wg = self.sb(ph, "wg", [128, 8, 2, DE], BF16)
        wu = self.sb(ph, "wu", [128, 8, 2, DE], BF16)
        wd = self.sb(ph, "wd", [128, 8, D], BF16)
        s_wg = [Slot() for _ in range(8)]
        s_wu = [Slot() for _ in range(8)]
        s_wd = [Slot() for _ in range(8)]
        tabg = self.w_eg.rearrange("l e (r two) c -> (l e r) (two c)", two=2)
        tabu = self.w_eu.rearrange("l e (r two) c -> (l e r) (two c)", two=2)
        tabd = self.w_ed.rearrange("l e r c -> (l e r) c")
        xb = [self.sb(ph, f"xb{i}", [128, D], BF16) for i in range(2)]
        s_xb = [Slot(), Slot()]
        xbT = [self.sb(ph, f"xbT{i}", [128, KC, 128], BF16) for i in range(2)]
        s_xbT = [Slot(), Slot()]
        sa = [self.sb(ph, f"sab{i}", [128, 512], F32) for i in range(2)]
        s_sa = [Slot(), Slot()]
        act = self.sb(ph, "actb", [128, DE], BF16)
        s_act = Slot()
        actT = self.sb(ph, "actT", [128, 8, 128], BF16)
        s_actT = Slot()
        ysb = [self.sb(ph, f"ysb{i}", [128, D], F32) for i in range(2)]
        s_ysb = [Slot(), Slot()]
        pv = [self.pb[i][:].bitcast(BF16) for i in range(8)]
        if SKIP:
            if not hasattr(self, "_bc_regs"):
                self._bc_regs = (self.nc.gpsimd.to_reg(NE * D - 1), self.nc.gpsimd.to_reg(NE * DE - 1))
            kw_g = dict(bounds_check=self._bc_regs[0], oob_is_err=False)
            kw_d = dict(bounds_check=self._bc_regs[1], oob_is_err=False)
        else:
            kw_g, kw_d = {}, {}

        def load_xb(b):
            self.dma("sp", xb[b % 2][:], self.buf_d[b * 128:(b + 1) * 128, :], writes=(s_xb[b % 2],))

        load_xb(0)
        for b in range(NBLK):
            r = b % 2
            if b + 1 < NBLK:
                load_xb(b + 1)
            for k in range(8):
                eo = (l * NE * DE + k * 128) * 2 * DE
                self.idma(wg[:, k].rearrange("p two c -> p (two c)"), tabg, sp["idxD_i"][:, b:b + 1], gather=True,
                          writes=(s_wg[k],), element_offset=eo, **kw_d)
                self.idma(wu[:, k].rearrange("p two c -> p (two c)"), tabu, sp["idxD_i"][:, b:b + 1], gather=True,
                          writes=(s_wu[k],), element_offset=eo, **kw_d)
            for k in range(8):
                eo = (l * NE * DE + k * 128) * D
                self.idma(wd[:, k, :], tabd, sp["idxD_i"][:, b:b + 1], gather=True, writes=(s_wd[k],),
                          element_offset=eo, **kw_d)
            for g in range(2):
                bk = 4 + g
                for j in range(8):
                    k = g * 8 + j
                    k2, par = k // 2, k % 2
                    self.op("pe", lambda E: E.transpose(pv[bk][:, j * 128:(j + 1) * 128],
                                                        xb[r][:, k2 * 256 + par:(k2 + 1) * 256:2],
                                                        self.identbf[:]),
                            reads=(s_xb[r], self.s_const), writes=(self.pbs[bk],), signal=(j == 7))
                src = pv[bk][:].rearrange("p (j t) -> p j t", j=8)
                self.op("dve", lambda E: E.tensor_copy(xbT[r][:, g * 8:(g + 1) * 8, :], src),
                        reads=(self.pbs[bk],), writes=(s_xbT[r],))
            for k in range(KC):
                for which, (wt, sw) in enumerate(((wg, s_wg), (wu, s_wu))):
                    for n in range(2):
                        bk = which * 2 + n
                        self.op("pe", lambda E: E.matmul(self.pb[bk][:], xbT[r][:, k, :],
                                                         wt[:, k // 2, k % 2, n * 512:(n + 1) * 512],
                                                         start=(k == 0), stop=(k == KC - 1)),
                                reads=(s_xbT[r], sw[k // 2]), writes=(self.pbs[bk],),
                                signal=(k == KC - 1 or (which == 1 and n == 1)))
            for n in range(2):
                self.op("act", lambda E: E.activation(out=sa[n][:], in_=self.pb[n][:], func=AF.Silu),
                        reads=(self.pbs[n],), writes=(s_sa[n],))
                self.op("dve", lambda E: E.tensor_tensor(out=act[:, n * 512:(n + 1) * 512], in0=self.pb[2 + n][:],
                                                         in1=sa[n][:], op=ALU.mult),
                        reads=(self.pbs[2 + n], s_sa[n]), writes=(s_act,))
            for j in range(8):
                self.op("pe", lambda E: E.transpose(pv[6][:, j * 128:(j + 1) * 128], act[:, j * 128:(j + 1) * 128],
                                                    self.identbf[:]),
                        reads=(s_act, self.s_const), writes=(self.pbs[6],), signal=(j == 7))
            self.op("dve", lambda E: E.tensor_copy(actT[:], pv[6][:].rearrange("p (j t) -> p j t", j=8)),
                    reads=(self.pbs[6],), writes=(s_actT,))
            for k in range(8):
                for n in range(4):
                    bk = 4 + n
                    self.op("pe", lambda E: E.matmul(self.pb[bk][:], actT[:, k, :], wd[:, k, n * 512:(n + 1) * 512],
                                                     start=(k == 0), stop=(k == 7)),
                            reads=(s_actT, s_wd[k]), writes=(self.pbs[bk],), signal=(k == 7))
            for n in range(4):
                bk = 4 + n
                ns = slice(n * 512, (n + 1) * 512)
                self.op("dve", lambda E: E.tensor_copy(ysb[r][:, ns], self.pb[bk][:]),
                        reads=(self.pbs[bk],), writes=(s_ysb[r],))
            self.dma("sp", self.ybuf_d[b * 128:(b + 1) * 128, :], ysb[r][:], reads=(s_ysb[r],))

    def phase_combine(self, ph, l, sp):
        G2 = self.sb(ph, "G2c", [128, D], F32)
        s_G2 = Slot()
        self.dma("sp", G2[:], self.mod_d[l:l + 1, 5 * D:6 * D].partition_broadcast(128), writes=(s_G2,))
        ylo = [self.sb(ph, f"ylo{i}", [128, D], F32) for i in range(2)]
        yhi = [self.sb(ph, f"yhi{i}", [128, D], F32) for i in range(2)]
        xt = [self.sb(ph, f"xc{i}", [128, D], F32) for i in range(2)]
        s_lo, s_hi, s_xt = [Slot(), Slot()], [Slot(), Slot()], [Slot(), Slot()]
        for t in range(NT):
            b = t % 2
            self.idma(ylo[b][:], self.ybuf_d, sp["dlo_i"][:, t:t + 1], gather=True, writes=(s_lo[b],))
            self.idma(yhi[b][:], self.ybuf_d, sp["dhi_i"][:, t:t + 1], gather=True, writes=(s_hi[b],))
            self.dma("sp", xt[b][:], self.xres[t * 128:(t + 1) * 128, :], writes=(s_xt[b],))
            self.op("dve", lambda E: E.tensor_scalar(out=ylo[b][:], in0=ylo[b][:], scalar1=sp["wlo"][:, t:t + 1], scalar2=None,
                                                     op0=ALU.mult), writes=(s_lo[b],))
            self.op("dve", lambda E: E.scalar_tensor_tensor(out=ylo[b][:], in0=yhi[b][:], scalar=sp["whi"][:, t:t + 1],
                                                            in1=ylo[b][:], op0=ALU.mult, op1=ALU.add),
                    reads=(s_hi[b],), writes=(s_lo[b],))
            self.op("dve", lambda E: E.tensor_tensor(out=ylo[b][:], in0=ylo[b][:], in1=G2[:], op=ALU.mult),
                    reads=(s_G2,), writes=(s_lo[b],))
            self.op("dve", lambda E: E.tensor_tensor(out=xt[b][:], in0=ylo[b][:], in1=xt[b][:], op=ALU.add),
                    reads=(s_lo[b],), writes=(s_xt[b],))
            self.dma("sp", self.xres[t * 128:(t + 1) * 128, :], xt[b][:], reads=(s_xt[b],))

    def phase_final(self):
        with contextlib.ExitStack() as ph:
            Gf = self.sb(ph, "Gf", [128, D], F32)
            s_G = Slot()
            self.dma("sp", Gf[:], self.norm_final.partition_broadcast(128), writes=(s_G,))
            xt = [self.sb(ph, f"xf{i}", [128, D], F32) for i in range(2)]
            s_xt = [Slot(), Slot()]
            yo = [self.sb(ph, f"yo{i}", [128, D], F32) for i in range(2)]
            s_yo = [Slot(), Slot()]
            junk = self.sb(ph, "junkf", [128, D], BF16)
            s_junk = Slot()
            ss = [self.sb(ph, f"ssf{i}", [128, 1], F32) for i in range(2)]
            s_ss = [Slot(), Slot()]
            self.dma("sp", xt[0][:], self.xres[0:128, :], writes=(s_xt[0],))
            for t in range(NT):
                b = t % 2
                if t + 1 < NT:
                    self.dma("sp", xt[1 - b][:], self.xres[(t + 1) * 128:(t + 2) * 128, :], writes=(s_xt[1 - b],))
                self.op("act", lambda E: E.activation(out=junk[:], in_=xt[b][:], func=AF.Square, accum_out=ss[b][:]),
                        reads=(s_xt[b],), writes=(s_junk, s_ss[b]))
                self.op("dve", lambda E: E.tensor_scalar(out=ss[b][:], in0=ss[b][:], scalar1=1.0 / D, scalar2=EPS,
                                                         op0=ALU.mult, op1=ALU.add), writes=(s_ss[b],))
                self.op("act", lambda E: E.activation(out=ss[b][:], in_=ss[b][:], func=AF.Sqrt), writes=(s_ss[b],))
                self.op("dve", lambda E: E.reciprocal(out=ss[b][:], in_=ss[b][:]), writes=(s_ss[b],))
                self.op("dve", lambda E: E.scalar_tensor_tensor(out=yo[b][:], in0=xt[b][:], scalar=ss[b][:, 0:1], in1=Gf[:],
                                                                op0=ALU.mult, op1=ALU.mult),
                        reads=(s_xt[b], s_ss[b], s_G), writes=(s_yo[b],))
                self.dma("sp", self.out[t * 128:(t + 1) * 128, :], yo[b][:], reads=(s_yo[b],))
        self.barrier()


_WNAMES = ["w_ada", "b_ada", "norm_mix", "norm_moe", "w_in", "b_forget", "w_pool", "pool_scale", "w_branch", "w_gate",
           "b_gate", "w_out", "w_router", "w_exp_gate", "w_exp_up", "w_exp_down"]


def make_in_maps(inputs, cores):
    f = lambda a: np.ascontiguousarray(np.asarray(a, dtype=np.float32))
    shared = {n: f(inputs[n]) for n in _WNAMES}
    shared["b_router"] = f(inputs["b_router"]).reshape(1, NE)
    shared["norm_final"] = f(inputs["norm_final"]).reshape(1, D)
    x = f(inputs["x"])
    c = f(inputs["c"])
    maps = []
    for b in cores:
        m = dict(shared)
        m["x"] = np.ascontiguousarray(x[b])
        m["c"] = np.ascontiguousarray(c[b:b + 1])
        maps.append(m)
    return maps


def kernel(**inputs):
    prog = Prog(depth=2)
    nc = prog.build()
    maps = make_in_maps(inputs, list(range(8)))
    res = run_bass_kernel_spmd(nc, maps, core_ids=list(range(8)))
    return np.stack([r["out"] for r in res.results], axis=0).astype(np.float32)
```
